# Optimizing a Trainium2 kernel written in Bass

```python
import math
import jax, jax.numpy as jnp
from jax import lax
import numpy as np

D_MODEL = 1024
BATCH = 4
SEQ = 8192
DEPTH = 1

D_MIX = D_MODEL
DA_HEADS = 4
DA_HEAD_DIM = 64
DA_VDIM = 2 * DA_HEAD_DIM
DA_WIDTH = DA_HEADS * DA_VDIM
Q_BLOCK = 128
ROPE_THETA = 10000.0
LAMBDA_STD = 0.1
GM_HEADS = 4
GM_HEAD_DIM = 128
GM_WIDTH = GM_HEADS * GM_HEAD_DIM
GM_CHUNK = 128
IN_COLS = 3 * DA_WIDTH + 2 * GM_WIDTH
N_GROUPS = 4
EXPERTS_PER_GROUP = 8
N_EXPERTS = N_GROUPS * EXPERTS_PER_GROUP
TOP_K = 2
D_EXPERT = 512
MOE_BLOCK = 128
PLE_DIM = 256
EPS = 1e-6

kernel_name = "hybrid_diffattn_gmlp_hmoe_block"


def rms_norm(x, g):
    xf = x.astype(jnp.float32)
    y = xf * lax.rsqrt(jnp.mean(xf * xf, axis=-1, keepdims=True) + EPS)
    return (y * g.astype(jnp.float32)).astype(x.dtype)


def layer_norm(x, g, b):
    xf = x.astype(jnp.float32)
    mu = jnp.mean(xf, axis=-1, keepdims=True)
    xc = xf - mu
    y = xc * lax.rsqrt(jnp.mean(xc * xc, axis=-1, keepdims=True) + EPS)
    return (y * g.astype(jnp.float32) + b.astype(jnp.float32)).astype(x.dtype)


def rope_tables(seq, dim):
    inv = 1.0 / (ROPE_THETA ** (jnp.arange(0, dim, 2, dtype=jnp.float32) / dim))
    ang = jnp.arange(seq, dtype=jnp.float32)[:, None] * inv[None, :]
    ang = jnp.concatenate([ang, ang], axis=-1)
    return jnp.cos(ang), jnp.sin(ang)


def apply_rope(x, cos, sin):
    half = x.shape[-1] // 2
    x1, x2 = x[..., :half], x[..., half:]
    rot = jnp.concatenate([-x2, x1], axis=-1)
    c = cos[:, None, None, :]
    s = sin[:, None, None, :]
    return (x * c + rot * s).astype(x.dtype)


def diff_attention(q, k, v, lq1, lk1, lq2, lk2, subln_g, lambda_init):
    B, S, _ = q.shape
    nq = S // Q_BLOCK
    cos, sin = rope_tables(S, DA_HEAD_DIM)
    q = apply_rope(q.reshape(B, S, DA_HEADS, 2, DA_HEAD_DIM), cos, sin)
    k = apply_rope(k.reshape(B, S, DA_HEADS, 2, DA_HEAD_DIM), cos, sin)
    v = v.reshape(B, S, DA_HEADS, DA_VDIM).transpose(0, 2, 1, 3)
    kt = k.transpose(0, 2, 3, 1, 4)
    qb = q.reshape(B, nq, Q_BLOCK, DA_HEADS, 2, DA_HEAD_DIM).transpose(1, 0, 3, 4, 2, 5)
    lam = (jnp.exp(jnp.sum(lq1.astype(jnp.float32) * lk1.astype(jnp.float32)))
           - jnp.exp(jnp.sum(lq2.astype(jnp.float32) * lk2.astype(jnp.float32)))
           + lambda_init)
    scale = 1.0 / math.sqrt(DA_HEAD_DIM)
    key_pos = jnp.arange(S)

    def block(args):
        q_blk, c = args
        s = jnp.einsum('bhmqd,bhmkd->bhmqk', q_blk, kt).astype(jnp.float32) * scale
        q_pos = c * Q_BLOCK + jnp.arange(Q_BLOCK)
        mask = key_pos[None, :] <= q_pos[:, None]
        s = jnp.where(mask, s, -jnp.inf)
        a = jax.nn.softmax(s, axis=-1)
        amap = a[:, :, 0] - lam * a[:, :, 1]
        return jnp.einsum('bhqk,bhkd->bhqd', amap.astype(v.dtype), v)

    o = lax.map(block, (qb, jnp.arange(nq)))
    o = o.transpose(1, 0, 3, 2, 4)
    o = rms_norm(o, subln_g) * (1.0 - lambda_init)
    return o.reshape(B, S, DA_WIDTH).astype(q.dtype)


def gmlp_spatial(z, ln_g, ln_b, ws, bs, out_g):
    B, S, _ = z.shape
    nc = S // GM_CHUNK
    a = jax.nn.gelu(z)
    u, vv = a[..., :GM_WIDTH], a[..., GM_WIDTH:]
    vv = layer_norm(vv, ln_g, ln_b)
    vv = vv.reshape(B, nc, GM_CHUNK, GM_HEADS, GM_HEAD_DIM)
    causal = jnp.tril(jnp.ones((GM_CHUNK, GM_CHUNK), dtype=bool))
    w = jnp.where(causal[None], ws, jnp.zeros_like(ws))
    mixed = jnp.einsum('hts,bcshd->bcthd', w, vv) + bs.T[:, :, None]
    out = u.reshape(B, nc, GM_CHUNK, GM_HEADS, GM_HEAD_DIM) * mixed
    return rms_norm(out.reshape(B, S, GM_WIDTH), out_g)


def hier_moe(h, w_grp, w_exp, w_g, w_u, w_d):
    B, S, D = h.shape
    T = B * S
    t = h.reshape(T, D)
    tok = jnp.arange(T)
    grp_logits = (t @ w_grp).astype(jnp.float32)
    grp_prob = jax.nn.softmax(grp_logits, axis=-1)
    g_idx = jnp.argmax(grp_logits, axis=-1)
    g_gate = grp_prob[tok, g_idx]
    exp_logits = (t @ w_exp).astype(jnp.float32).reshape(T, N_GROUPS, EXPERTS_PER_GROUP)
    in_grp = exp_logits[tok, g_idx]
    top_val, top_loc = lax.top_k(in_grp, TOP_K)
    gate = jax.nn.softmax(top_val, axis=-1) * g_gate[:, None]
    e_id = g_idx[:, None] * EXPERTS_PER_GROUP + top_loc

    n_assign = T * TOP_K
    flat_e = e_id.reshape(-1)
    flat_w = gate.reshape(-1)
    flat_tok = jnp.repeat(tok, TOP_K)
    order = jnp.argsort(flat_e)
    se, stok, sw = flat_e[order], flat_tok[order], flat_w[order]
    counts = jnp.bincount(flat_e, length=N_EXPERTS)
    start = jnp.cumsum(counts) - counts
    padded = ((counts + MOE_BLOCK - 1) // MOE_BLOCK) * MOE_BLOCK
    pend = jnp.cumsum(padded)
    pstart = pend - padded
    dest = pstart[se] + (jnp.arange(n_assign) - start[se])
    n_rows = n_assign + N_EXPERTS * MOE_BLOCK
    n_blocks = n_rows // MOE_BLOCK
    buf = jnp.zeros((n_rows, D), t.dtype).at[dest].set(t[stok])
    blk_exp = jnp.clip(jnp.searchsorted(pend, jnp.arange(n_blocks) * MOE_BLOCK, side='right'),
                       0, N_EXPERTS - 1)

    def expert_block(args):
        xb, e = args
        return (jax.nn.silu(xb @ w_g[e]) * (xb @ w_u[e])) @ w_d[e]

    ybuf = lax.map(expert_block, (buf.reshape(n_blocks, MOE_BLOCK, D), blk_exp)).reshape(n_rows, D)
    y = ybuf[dest] * sw[:, None].astype(t.dtype)
    out = jnp.zeros((T, D), t.dtype).at[stok].add(y)
    return out.reshape(B, S, D)


def setup_inputs(seed: int = 0) -> dict:
    key = jax.random.key(seed)
    ks = jax.random.split(key, 26)
    f32 = jnp.float32
    nrm = lambda k, shape, s: (jax.random.normal(k, shape, f32) * s)
    gain = lambda k, shape: 1.0 + 0.02 * jax.random.normal(k, shape, f32)
    L, D = DEPTH, D_MODEL
    return {
        "x": jax.random.normal(ks[0], (BATCH, SEQ, D), f32),
        "p": jax.random.normal(ks[1], (DEPTH, BATCH, SEQ, PLE_DIM), f32),
        "attn_norm": gain(ks[2], (L, D)),
        "w_in": nrm(ks[3], (L, D, IN_COLS), D ** -0.5),
        "lambda_q1": nrm(ks[4], (L, DA_HEAD_DIM), LAMBDA_STD),
        "lambda_k1": nrm(ks[5], (L, DA_HEAD_DIM), LAMBDA_STD),
        "lambda_q2": nrm(ks[6], (L, DA_HEAD_DIM), LAMBDA_STD),
        "lambda_k2": nrm(ks[7], (L, DA_HEAD_DIM), LAMBDA_STD),
        "diff_subln": gain(ks[8], (L, DA_VDIM)),
        "gm_ln_gain": gain(ks[9], (L, GM_WIDTH)),
        "gm_ln_bias": nrm(ks[10], (L, GM_WIDTH), 0.02),
        "gm_spatial_w": nrm(ks[11], (L, GM_HEADS, GM_CHUNK, GM_CHUNK), GM_CHUNK ** -0.5),
        "gm_spatial_b": gain(ks[12], (L, GM_HEADS, GM_CHUNK)),
        "gm_out_norm": gain(ks[13], (L, GM_WIDTH)),
        "w_out": nrm(ks[14], (L, DA_WIDTH + GM_WIDTH, D), (DA_WIDTH + GM_WIDTH) ** -0.5),
        "moe_norm": gain(ks[15], (L, D)),
        "w_group_router": nrm(ks[16], (L, D, N_GROUPS), D ** -0.5),
        "w_expert_router": nrm(ks[17], (L, D, N_EXPERTS), D ** -0.5),
        "w_expert_gate": nrm(ks[18], (L, N_EXPERTS, D, D_EXPERT), D ** -0.5),
        "w_expert_up": nrm(ks[19], (L, N_EXPERTS, D, D_EXPERT), D ** -0.5),
        "w_expert_down": nrm(ks[20], (L, N_EXPERTS, D_EXPERT, D), D_EXPERT ** -0.5),
        "ple_norm": gain(ks[21], (L, D)),
        "w_ple_gate": nrm(ks[22], (L, D, D), D ** -0.5),
        "b_ple_gate": nrm(ks[23], (L, D), 0.02),
        "w_ple_proj": nrm(ks[24], (L, PLE_DIM, D), PLE_DIM ** -0.5),
        "final_norm": gain(ks[25], (D,)),
    }


def reference(x, p, attn_norm, w_in, lambda_q1, lambda_k1, lambda_q2, lambda_k2, diff_subln,
              gm_ln_gain, gm_ln_bias, gm_spatial_w, gm_spatial_b, gm_out_norm, w_out,
              moe_norm, w_group_router, w_expert_router, w_expert_gate, w_expert_up,
              w_expert_down, ple_norm, w_ple_gate, b_ple_gate, w_ple_proj, final_norm):
    for i in range(DEPTH):
        lambda_init = 0.8 - 0.6 * math.exp(-0.3 * i)
        h = rms_norm(x, attn_norm[i])
        z = h @ w_in[i]
        q = z[..., :DA_WIDTH]
        k = z[..., DA_WIDTH:2 * DA_WIDTH]
        v = z[..., 2 * DA_WIDTH:3 * DA_WIDTH]
        zg = z[..., 3 * DA_WIDTH:]
        o_da = diff_attention(q, k, v, lambda_q1[i], lambda_k1[i], lambda_q2[i], lambda_k2[i],
                              diff_subln[i], lambda_init)
        o_gm = gmlp_spatial(zg, gm_ln_gain[i], gm_ln_bias[i], gm_spatial_w[i],
                            gm_spatial_b[i], gm_out_norm[i])
        x = x + jnp.concatenate([o_da, o_gm], axis=-1) @ w_out[i]
        x = x + hier_moe(rms_norm(x, moe_norm[i]), w_group_router[i], w_expert_router[i],
                         w_expert_gate[i], w_expert_up[i], w_expert_down[i])
        gate = jax.nn.sigmoid(rms_norm(x, ple_norm[i]) @ w_ple_gate[i] + b_ple_gate[i])
        x = x + gate * (p[i] @ w_ple_proj[i])
    return rms_norm(x, final_norm)
```

```python
import contextlib
import numpy as np
import concourse.bass as bass
import concourse.mybir as mybir
from concourse.bass_utils import run_bass_kernel_spmd

F32 = mybir.dt.float32
BF16 = mybir.dt.bfloat16
AF = mybir.ActivationFunctionType
ALU = mybir.AluOpType
AX = mybir.AxisListType

D = 1024
S = 8192
NOWN = 4096
NBLK = 32
EPS = 1e-6
ENGS = ["sync", "act", "dve", "pool", "pe"]
NPOOL = 8
NSPLIT = 4
LAMBDA_INIT = 0.2


class Op:
    __slots__ = ("eng", "fn", "deps", "dma", "needs_sig", "sigval", "pool", "val", "prev_val")

    def __init__(self, eng, fn, dma):
        self.eng = eng
        self.fn = fn
        self.dma = dma
        self.deps = ()
        self.needs_sig = False
        self.sigval = 0
        self.pool = 0
        self.val = 0
        self.prev_val = 0


class Prog:
    def __init__(self, nc):
        self.nc = nc
        self.ops = {e: [] for e in ENGS}
        self.lastw = {}
        self.readers = {}
        self.dma_uses = {e: [0] * NPOOL for e in ENGS}
        self.dma_rr = {e: 0 for e in ENGS}

    def add(self, eng, fn, r=(), w=(), dma=False):
        op = Op(eng, fn, dma)
        deps = set()
        for k in r:
            lw = self.lastw.get(k)
            if lw is not None:
                deps.add(lw)
        for k in w:
            lw = self.lastw.get(k)
            if lw is not None:
                deps.add(lw)
            for rd in self.readers.get(k, ()):
                deps.add(rd)
        deps.discard(op)
        fl = []
        for d in deps:
            if (not d.dma) and (not dma) and d.eng == eng == "pe":
                continue
            fl.append(d)
            if not d.dma:
                d.needs_sig = True
        op.deps = fl
        for k in r:
            self.readers.setdefault(k, []).append(op)
        for k in w:
            self.lastw[k] = op
            self.readers[k] = []
        if dma:
            i = self.dma_rr[eng]
            self.dma_rr[eng] = (i + 1) % NPOOL
            op.pool = i
            op.prev_val = 16 * self.dma_uses[eng][i]
            self.dma_uses[eng][i] += 1
            op.val = 16 * self.dma_uses[eng][i]
        self.ops[eng].append(op)
        return op

    def I(self, eng, meth, r, w, *args, **kw):
        def fn(e):
            return getattr(e, meth)(*args, **kw)
        return self.add(eng, fn, r, w)

    def dma(self, eng, out, in_, r=(), w=()):
        def fn(e):
            return e.dma_start(out=out, in_=in_)
        return self.add(eng, fn, r, w, dma=True)

    def wait(self, eng, r):
        return self.add(eng, None, r, ())

    def barrier(self):
        deps = []
        for e in ENGS:
            seen_c = False
            for op in reversed(self.ops[e]):
                if op.fn is None:
                    if op.eng == "__bar__":
                        break
                    continue
                if op.dma:
                    deps.append(op)
                elif not seen_c:
                    seen_c = True
                    op.needs_sig = True
                    deps.append(op)
        for e in ENGS:
            op = Op(e, None, False)
            op.deps = list(deps)
            self.ops[e].append(op)

    def emit(self):
        nc = self.nc
        for e in ENGS:
            cnt = 0
            for op in self.ops[e]:
                if op.dma or op.fn is None:
                    continue
                if op.needs_sig:
                    cnt += 1
                    op.sigval = cnt
        with contextlib.ExitStack() as st:
            csem = {e: st.enter_context(nc.semaphore("c_" + e)) for e in ENGS}
            dsem = {}
            for e in ENGS:
                if any(o.dma for o in self.ops[e]):
                    for i in range(NPOOL):
                        dsem[(e, i)] = st.enter_context(nc.semaphore("d_%s_%d" % (e, i)))
            block = st.enter_context(nc.Block())

            def run(e, eh):
                waited = {}
                for op in self.ops[e]:
                    need = {}
                    for d in op.deps:
                        if d.dma:
                            key = (d.eng, d.pool)
                            val = d.val
                        else:
                            key = d.eng
                            val = d.sigval
                        if need.get(key, 0) < val:
                            need[key] = val
                    if op.dma and op.prev_val > 0:
                        key = (e, op.pool)
                        if need.get(key, 0) < op.prev_val:
                            need[key] = op.prev_val
                    for key, val in need.items():
                        if waited.get(key, 0) < val:
                            sem = dsem[key] if isinstance(key, tuple) else csem[key]
                            eh.wait_ge(sem, val)
                            waited[key] = val
                    if op.fn is None:
                        continue
                    ins = op.fn(eh)
                    if op.dma:
                        ins.then_inc(dsem[(e, op.pool)], 16)
                    elif op.needs_sig:
                        ins.then_inc(csem[e], 1)

            block.sync(lambda eh: run("sync", eh))
            block.scalar(lambda eh: run("act", eh))
            block.vector(lambda eh: run("dve", eh))
            block.gpsimd(lambda eh: run("pool", eh))
            block.tensor(lambda eh: run("pe", eh))


class Arena:
    def __init__(self, nc, nbytes):
        self.t = nc.alloc_sbuf_tensor("arena", [128, nbytes // 4], F32)
        self.nbytes = nbytes
        self.off = 0
        self.marks = []

    def push(self):
        self.marks.append(self.off)

    def pop(self):
        self.off = self.marks.pop()

    def alloc(self, shape, dtype):
        esz = 2 if dtype == BF16 else 4
        n = 1
        for s in shape[1:]:
            n *= s
        nb = (n * esz + 31) // 32 * 32
        assert self.off + nb <= self.nbytes, ("sbuf arena overflow", self.off, nb)
        ap = self.t[:, self.off // 4:(self.off + nb) // 4]
        if dtype != F32:
            ap = ap.bitcast(dtype)
        ap = ap[0:shape[0], 0:n]
        self.off += nb
        if len(shape) == 3:
            ap = ap.rearrange("p (a b) -> p a b", b=shape[2])
        elif len(shape) == 4:
            ap = ap.rearrange("p (a b c) -> p a b c", b=shape[2], c=shape[3])
        return ap


def build_program(debug=None):
    nc = bass.Bass("TRN2", target_bir_lowering=False)
    P = Prog(nc)
    I = P.I

    def din(name, shape, dt=F32):
        return nc.dram_tensor(name, list(shape), dt, kind="ExternalInput").ap()

    x_full = din("x_full", [S, D])
    x_own = din("x_own", [NOWN, D])
    w_kv = din("w_kv", [D, 1536])
    w_q = din("w_q", [D, 2048])
    cs_full = din("cs_full", [2, 128, S])
    cs_own = din("cs_own", [2, 128, NOWN])
    masks_d = din("masks", [3, 128, 128])
    ident_d = din("ident", [128, 128])
    wsT_d = din("wsT", [4, 128, 128])
    bsT_d = din("bsT", [128, 4])
    vec_d = {n: din(n, [1, sz]) for n, sz in [
        ("attn_norm", D), ("moe_norm", D), ("ple_norm", D), ("final_norm", D), ("b_ple_gate", D),
        ("gm_ln_gain", 512), ("gm_ln_bias", 512), ("gm_out_norm", 512), ("diff_subln", 128),
        ("lambda_q1", 64), ("lambda_k1", 64), ("lambda_q2", 64), ("lambda_k2", 64)]}
    w_out_d = din("w_out", [D, D])
    w_r_d = din("w_router", [D, 36])
    out_d = nc.dram_tensor("out", [NOWN, D], F32, kind="ExternalOutput").ap()

    kt_d = nc.dram_tensor("kt_scr", [128, 4, S], BF16, kind="Internal").ap()
    v_d = nc.dram_tensor("v_scr", [4, 128, 64 * 130], BF16, kind="Internal").ap()
    qt_d = nc.dram_tensor("qt_scr", [128, 4, NOWN], BF16, kind="Internal").ap()
    x1_d = nc.dram_tensor("x1_scr", [NOWN, D], F32, kind="Internal").ap()
    hnT_d = nc.dram_tensor("hnT_scr", [NBLK, 128, D], BF16, kind="Internal").ap()
    dbg_out = {}
    if debug:
        for n, shp in debug.items():
            if n.startswith("_"):
                continue
            dbg_out[n] = nc.dram_tensor("dbg_" + n, list(shp), F32, kind="ExternalOutput").ap()
    stop_after = (debug or {}).get("_stop", None)
    fin_keys = []

    A = Arena(nc, 206 * 1024)
    psum = nc.alloc_psum_tensor("psum", [128, 4096], F32)

    def bank(b, n=512):
        return psum[:, b * 512:b * 512 + n]

    def bank_bf(b):
        return psum[:, b * 512:(b + 1) * 512].bitcast(BF16)

    def bk(b):
        return ("bank", b)

    ident = A.alloc([128, 128], BF16)
    P.dma("pool", ident, ident_d, w=["ident"])
    bc = {}
    for n, ap in vec_d.items():
        sz = ap.shape[1]
        t = A.alloc([128, sz], F32)
        bc[n] = t
        P.dma("sync", t, ap.broadcast_to([128, sz]), w=["bc_" + n])
    gates = A.alloc([128, NBLK, 32], F32)
    stat = A.alloc([128, 256], F32)
    stat_i = [0]

    def st1(n=1):
        i = stat_i[0]
        if i + n > 256:
            i = 0
        stat_i[0] = i + n
        return stat[:, i:i + n], ("stat", i)

    junk = A.alloc([128, 1024], BF16)

    lam_t = A.alloc([128, 8], F32)
    lj = A.alloc([128, 64], F32)
    I("pool", "memset", [], ["lam"], lam_t, 0.0)
    for i, (a, b) in enumerate([("lambda_q1", "lambda_k1"), ("lambda_q2", "lambda_k2")]):
        I("dve", "tensor_tensor", ["bc_" + a, "bc_" + b], ["lj"], out=lj, in0=bc[a], in1=bc[b], op=ALU.mult)
        I("dve", "reduce_sum", ["lj", "lam"], ["lam"], out=lam_t[:, i:i + 1], in_=lj, axis=AX.X)
    I("act", "activation", ["lam"], ["lam"], out=lam_t[:, 2:4], in_=lam_t[:, 0:2], func=AF.Exp)
    I("dve", "tensor_tensor", ["lam"], ["lam"], out=lam_t[:, 4:5], in0=lam_t[:, 3:4], in1=lam_t[:, 2:3], op=ALU.subtract)
    I("dve", "tensor_scalar", ["lam"], ["lam"], out=lam_t[:, 5:6], in0=lam_t[:, 4:5], scalar1=-LAMBDA_INIT, scalar2=None,
      op0=ALU.add)
    neglam = lam_t[:, 5:6]
    I("dve", "tensor_scalar", ["bc_diff_subln"], ["bc_diff_subln"], out=bc["diff_subln"], in0=bc["diff_subln"],
      scalar1=1.0 - LAMBDA_INIT, scalar2=None, op0=ALU.mult)

    def rstd_ops(ssq, ssq_k, n, eps=EPS):
        t1, k1 = st1()
        t2, k2 = st1()
        t3, k3 = st1()
        I("dve", "tensor_scalar", [ssq_k], [k1], out=t1, in0=ssq, scalar1=1.0 / n, scalar2=eps, op0=ALU.mult, op1=ALU.add)
        I("act", "sqrt", [k1], [k2], out=t2, in_=t1)
        I("dve", "reciprocal", [k2], [k3], out=t3, in_=t2)
        return t3, k3

    def sumsq(src, src_k, bias=None, bias_k=None):
        s, k = st1()
        I("pool", "memset", [], [k], s, 0.0)
        n = src.shape[1]
        if bias is None:
            I("act", "activation", [src_k, k], [k, "junk"], out=junk[:, 0:n], in_=src, func=AF.Square, accum_out=s)
        else:
            I("act", "activation", [src_k, k, bias_k], [k, "junk"], out=junk[:, 0:n], in_=src, func=AF.Square, bias=bias,
              accum_out=s)
        return s, k

    def transpose_block(src, src_k, nch, dst, dst_k, tb, eng="act"):
        pt = bank_bf(tb)
        for c in range(nch):
            I("pe", "transpose", [src_k, "ident"], [bk(tb)], out=pt[:, c * 128:(c + 1) * 128], in_=src[:, c * 128:(c + 1) * 128],
              identity=ident)
        pv = pt[:, 0:nch * 128].rearrange("p (a b) -> p a b", b=128)
        if eng == "act":
            I("act", "copy", [bk(tb)], [dst_k], out=dst, in_=pv)
        else:
            I("dve", "tensor_copy", [bk(tb)], [dst_k], out=dst, in_=pv)

    def norm_transpose(xt, xt_k, gain, gain_k, hT_dst, hT_k, tb, hb, hb_k):
        ssq, ssq_k = sumsq(xt, xt_k)
        rstd, rk = rstd_ops(ssq, ssq_k, D)
        I("dve", "scalar_tensor_tensor", [xt_k, rk, gain_k], [hb_k], out=hb, in0=xt, scalar=rstd, in1=gain,
          op0=ALU.mult, op1=ALU.mult)
        transpose_block(hb, hb_k, 8, hT_dst, hT_k, tb)

    def mm(out, lhsT, rhs, start, stop, r, w, skip=False):
        if skip:
            I("pe", "matmul", r, w, out, lhsT=lhsT, rhs=rhs, start=start, stop=stop, skip_group_check=True)
        else:
            I("pe", "matmul", r, w, out, lhsT=lhsT, rhs=rhs, start=start, stop=stop)

    scr_keys = {"kt": [], "qt": [], "v": [], "x1": [], "hnT": []}

    def proj_phase(which):
        A.push()
        is_a = which == "A"
        ncols = 1536 if is_a else 2048
        wsrc = w_kv if is_a else w_q
        xsrc = x_full if is_a else x_own
        cs = cs_full if is_a else cs_own
        ngrp = 16 if is_a else 8
        dst_d = kt_d if is_a else qt_d
        skey = "kt" if is_a else "qt"
        wb = A.alloc([128, 8, ncols], BF16)
        wv = wsrc.rearrange("(kc p) c -> p kc c", p=128)
        for kc in range(8):
            P.dma("pool", wb[:, kc, :], wv[:, kc, :], w=[("wproj", kc)])
        xts = [A.alloc([128, D], F32) for _ in range(3)]
        hbs = [A.alloc([128, D], BF16) for _ in range(2)]
        hTs = [A.alloc([128, 8, 512], BF16) for _ in range(2)]
        cst = [A.alloc([128, 2, 512], F32) for _ in range(2)]
        t1s = [A.alloc([128, 512], F32) for _ in range(2)]
        t2s = [A.alloc([128, 512], F32) for _ in range(2)]
        stg = [A.alloc([128, 4, 512], BF16) for _ in range(2)]
        if is_a:
            vgs = [A.alloc([128, 4, 4, 130], BF16) for _ in range(2)]
            for i in range(2):
                I("pool", "memset", [], [("vg", i)], vgs[i], 1.0)
        else:
            wsT = A.alloc([128, 4, 128], BF16)
            wsf = A.alloc([128, 4, 128], F32)
            tri = A.alloc([128, 128], F32)
            bsT = A.alloc([128, 4], F32)
            P.dma("sync", wsf, wsT_d.rearrange("h s t -> s h t"), w=["wsf"])
            P.dma("sync", tri, masks_d[2], w=["tri"])
            P.dma("sync", bsT, bsT_d, w=["bsT"])
            for h in range(4):
                I("dve", "tensor_tensor", ["wsf", "tri"], ["wsT"], out=wsT[:, h, :], in0=wsf[:, h, :], in1=tri, op=ALU.mult)
            bufA = A.alloc([128, 1024], F32)
            bufB = A.alloc([128, 1024], F32)
            vn1 = A.alloc([128, 512], F32)
            vnb = A.alloc([128, 512], BF16)

        nblk = ngrp * 4

        def load_x(bi):
            if bi >= nblk:
                return
            sl = bi % 3
            P.dma("sync", xts[sl], xsrc[bi * 128:(bi + 1) * 128, :], w=[("xt", sl)])

        def load_cs(g):
            if g >= ngrp:
                return
            sl = g % 2
            P.dma("sync", cst[sl], cs[:, :, g * 512:(g + 1) * 512].rearrange("a p t -> p a t"), w=[("cs", sl)])

        load_x(0)
        load_x(1)
        load_cs(0)
        for g in range(ngrp):
            gs = g % 2
            hT = hTs[gs]
            load_cs(g + 1)
            for j in range(4):
                bi = g * 4 + j
                load_x(bi + 2)
                sl = bi % 3
                norm_transpose(xts[sl], ("xt", sl), bc["attn_norm"], "bc_attn_norm",
                               hT[:, :, j * 128:(j + 1) * 128], ("hT", gs, j), bi % 2, hbs[bi % 2], ("hb", bi % 2))
            hkeys = [("hT", gs, j) for j in range(4)]
            for h in range(4):
                pb = 2 + 2 * (h % 2)
                for which_c, b in ((0, pb), (1, pb + 1)):
                    c0 = which_c * 512 + h * 128
                    for kc in range(8):
                        mm(bank(b), wb[:, kc, c0:c0 + 128], hT[:, kc, :], kc == 0, kc == 7, hkeys + [("wproj", kc)], [bk(b)])
                ts = h % 2
                I("dve", "tensor_tensor", [bk(pb), ("cs", gs)], [("t1", ts)], out=t1s[ts], in0=bank(pb), in1=cst[gs][:, 0, :],
                  op=ALU.mult)
                I("dve", "tensor_tensor", [bk(pb + 1), ("cs", gs)], [("t2", ts)], out=t2s[ts], in0=bank(pb + 1),
                  in1=cst[gs][:, 1, :], op=ALU.mult)
                I("pool", "tensor_tensor", [("t1", ts), ("t2", ts)], [("stg", gs)], out=stg[gs][:, h, :], in0=t1s[ts],
                  in1=t2s[ts], op=ALU.add)
            k = (skey, g)
            scr_keys[skey].append(k)
            P.dma("sync", dst_d[:, :, g * 512:(g + 1) * 512], stg[gs], r=[("stg", gs)], w=[k])
            if is_a:
                for j in range(4):
                    b = 6 + (j % 2)
                    for kc in range(8):
                        mm(bank(b), hT[:, kc, j * 128:(j + 1) * 128], wb[:, kc, 1024:1536], kc == 0, kc == 7,
                           [("hT", gs, j), ("wproj", kc)], [bk(b)])
                    I("act", "copy", [bk(b)], [("vg", gs)], out=vgs[gs][:, j, :, 0:128],
                      in_=bank(b).rearrange("p (h d) -> p h d", d=128))
                for h in range(4):
                    k = ("v", g, h)
                    scr_keys["v"].append(k)
                    P.dma("sync", v_d[h][:, g * 520:(g + 1) * 520].rearrange("p (j c) -> p j c", c=130), vgs[gs][:, :, h, :],
                          r=[("vg", gs)], w=[k])
            else:
                for j in range(4):
                    bi = g * 4 + j
                    for half in range(2):
                        for kc in range(8):
                            mm(bank(6 + half), hT[:, kc, j * 128:(j + 1) * 128],
                               wb[:, kc, 1024 + half * 512:1536 + half * 512], kc == 0, kc == 7,
                               [("hT", gs, j), ("wproj", kc)], [bk(6 + half)])
                    zp = psum[:, 6 * 512:8 * 512]
                    zk = [bk(6), bk(7)]
                    I("act", "activation", zk + ["bufA"], ["bufA"], out=bufA, in_=zp, func=AF.Square)
                    I("dve", "tensor_scalar", ["bufA"], ["bufA"], out=bufA, in0=bufA, scalar1=0.044715, scalar2=1.0,
                      op0=ALU.mult, op1=ALU.add)
                    I("dve", "tensor_tensor", ["bufA"] + zk, ["bufA"], out=bufA, in0=bufA, in1=zp, op=ALU.mult)
                    I("act", "activation", ["bufA", "bufB"], ["bufB"], out=bufB, in_=bufA, func=AF.Sigmoid,
                      scale=1.5957691216057308)
                    I("dve", "tensor_tensor", ["bufB", "bufA"] + zk, ["bufA"], out=bufA, in0=bufB, in1=zp, op=ALU.mult)
                    ga = bufA
                    u = ga[:, 0:512]
                    vv = ga[:, 512:1024]
                    og = bufB[:, 0:512]
                    s1, s1k = st1()
                    I("dve", "reduce_sum", ["bufA"], [s1k], out=s1, in_=vv, axis=AX.X)
                    nm, nmk = st1()
                    I("dve", "tensor_scalar", [s1k], [nmk], out=nm, in0=s1, scalar1=-1.0 / 512, scalar2=None, op0=ALU.mult)
                    ssq, ssqk = sumsq(vv, "bufA", bias=nm, bias_k=nmk)
                    rstd, rk = rstd_ops(ssq, ssqk, 512)
                    I("dve", "tensor_scalar", ["bufA", nmk, rk], ["vn1"], out=vn1, in0=vv, scalar1=nm, scalar2=rstd,
                      op0=ALU.add, op1=ALU.mult)
                    I("pool", "tensor_tensor", ["vn1", "bc_gm_ln_gain"], ["vn1"], out=vn1, in0=vn1, in1=bc["gm_ln_gain"],
                      op=ALU.mult)
                    I("pool", "tensor_tensor", ["vn1", "bc_gm_ln_bias"], ["vnb"], out=vnb, in0=vn1, in1=bc["gm_ln_bias"],
                      op=ALU.add)
                    for h in range(4):
                        mm(bank(2)[:, h * 128:(h + 1) * 128], wsT[:, h, :], vnb[:, h * 128:(h + 1) * 128], True, True,
                           ["wsT", "vnb"], [bk(2)])
                    for h in range(4):
                        I("dve", "scalar_tensor_tensor", [bk(2), "bsT", "bufA", "bufB"], ["bufB"], out=og[:, h * 128:(h + 1) * 128],
                          in0=bank(2)[:, h * 128:(h + 1) * 128], scalar=bsT[:, h:h + 1], in1=u[:, h * 128:(h + 1) * 128],
                          op0=ALU.add, op1=ALU.mult)
                    ssq2, ssq2k = sumsq(og, "bufB")
                    rstd2, r2k = rstd_ops(ssq2, ssq2k, 512)
                    I("dve", "scalar_tensor_tensor", ["bufB", r2k, "bc_gm_out_norm"], [("mix", bi)], out=mix[:, bi, 512:1024],
                      in0=og, scalar=rstd2, in1=bc["gm_out_norm"], op0=ALU.mult, op1=ALU.mult)
        A.pop()

    def dump_mix():
        if "mix" in dbg_out:
            P.barrier()
            A.push()
            mf = A.alloc([128, D], F32)
            for bi in range(NBLK):
                I("dve", "tensor_copy", [("mix", bi)], ["mf"], out=mf, in_=mix[:, bi, :])
                P.dma("sync", dbg_out["mix"][bi * 128:(bi + 1) * 128, :], mf, r=["mf"], w=[("dbgmix", bi)])
                fin_keys.append(("dbgmix", bi))
            A.pop()

    def finish():
        P.wait("sync", fin_keys)
        P.emit()
        return nc

    proj_phase("A")
    P.barrier()
    A.push()
    mix = A.alloc([128, NBLK, D], BF16)
    proj_phase("B")
    if stop_after == "B":
        dump_mix()
        return finish()

    P.barrier()
    A.push()
    KT = [A.alloc([128, S], BF16) for _ in range(2)]
    VH = [A.alloc([128, 64, 130], BF16) for _ in range(2)]
    QT = [A.alloc([128, NOWN], BF16) for _ in range(2)]
    PT = [[A.alloc([128, 512], BF16) for _ in range(3)] for _ in range(2)]
    mk = A.alloc([128, 2, 128], BF16)
    P.dma("pool", mk, masks_d[0:2].rearrange("a k q -> k a q"), w=["mk"])
    t1c = A.alloc([128, 128], F32)
    odc = A.alloc([128, 128], F32)

    def load_head(h):
        if h >= 4:
            return
        sl = h % 2
        P.dma("sync", KT[sl], kt_d[:, h, :], r=scr_keys["kt"], w=[("KT", sl)])
        P.dma("sync", VH[sl], v_d[h].rearrange("p (k c) -> p k c", c=130), r=scr_keys["v"], w=[("VH", sl)])
        P.dma("sync", QT[sl], qt_d[:, h, :], r=scr_keys["qt"], w=[("QT", sl)])

    def oreg(c, m):
        idx = c * 2 + m
        b = 4 + idx // 3
        off = (idx % 3) * 130
        return psum[:, b * 512 + off:b * 512 + off + 130], ("oacc", idx)

    load_head(0)
    for h in range(4):
        load_head(h + 1)
        hs = h % 2
        kt, vh, qt = KT[hs], VH[hs], QT[hs]
        hk = [("KT", hs), ("QT", hs)]
        for g in range(8):
            nkb = 8 * g + 8
            steps = [(kb, m) for kb in range(nkb) for m in range(2)]

            def cmin_of(kb):
                j = kb - (nkb - 8)
                return (j // 2) if j > 0 else 0

            def qk(si):
                kb, m = steps[si]
                cmin = cmin_of(kb)
                b = si % 4
                q0 = g * 512 + cmin * 128
                mm(bank(b)[:, cmin * 128:512], kt[m * 64:(m + 1) * 64, kb * 128:(kb + 1) * 128],
                   qt[m * 64:(m + 1) * 64, q0:(g + 1) * 512], True, True, hk, [bk(b)])

            qk(0)
            qk(1)
            for si in range(len(steps)):
                kb, m = steps[si]
                if si + 2 < len(steps):
                    qk(si + 2)
                j = kb - (nkb - 8)
                cmin = cmin_of(kb)
                b = si % 4
                pslot = (si // 2) % 3
                pt = PT[m][pslot]
                ptk = ("PT", m, pslot)
                I("act", "activation", [bk(b)], [ptk], out=pt[:, cmin * 128:512], in_=bank(b)[:, cmin * 128:512],
                  func=AF.Exp, scale=0.125)
                for c in range(cmin, 4):
                    if j == 2 * c or j == 2 * c + 1:
                        mi = j - 2 * c
                        I("pool", "tensor_tensor", [ptk, "mk"], [ptk], out=pt[:, c * 128:(c + 1) * 128],
                          in0=pt[:, c * 128:(c + 1) * 128], in1=mk[:, mi, :], op=ALU.mult)
                for c in range(cmin, 4):
                    oa, ok = oreg(c, m)
                    last = (nkb - 8) + 2 * c + 1
                    first_in_bank = (kb == 0 and m == 0 and c in (0, 2, 3))
                    mm(oa[:, 0:129], pt[:, c * 128:(c + 1) * 128], vh[:, kb, 0:129], first_in_bank, kb == last,
                       [ptk, ("VH", hs)], [ok], skip=True)
            for c in range(4):
                bi = g * 4 + c
                o1, k1 = oreg(c, 0)
                o2, k2 = oreg(c, 1)
                ri1, ri1k = st1()
                ri2, ri2k = st1()
                I("dve", "reciprocal", [k1], [ri1k], out=ri1, in_=o1[:, 128:129])
                I("dve", "reciprocal", [k2], [ri2k], out=ri2, in_=o2[:, 128:129])
                nl, nlk = st1()
                I("dve", "tensor_scalar", [ri2k, "lam"], [nlk], out=nl, in0=ri2, scalar1=neglam, scalar2=None, op0=ALU.mult)
                I("dve", "tensor_scalar", [k1, ri1k], ["t1c"], out=t1c, in0=o1[:, 0:128], scalar1=ri1, scalar2=None, op0=ALU.mult)
                I("dve", "scalar_tensor_tensor", [k2, nlk, "t1c"], ["odc"], out=odc, in0=o2[:, 0:128], scalar=nl, in1=t1c,
                  op0=ALU.mult, op1=ALU.add)
                ssq, ssqk = sumsq(odc, "odc")
                rstd, rk = rstd_ops(ssq, ssqk, 128)
                I("dve", "scalar_tensor_tensor", ["odc", rk, "bc_diff_subln"], [("mix", bi)],
                  out=mix[:, bi, h * 128:(h + 1) * 128], in0=odc, scalar=rstd, in1=bc["diff_subln"], op0=ALU.mult, op1=ALU.mult)
    A.pop()
    dump_mix()
    if stop_after == "C":
        return finish()

    P.barrier()
    A.push()
    wo = A.alloc([128, 8, D], BF16)
    wov = w_out_d.rearrange("(kc p) c -> p kc c", p=128)
    for kc in range(8):
        P.dma("pool", wo[:, kc, :], wov[:, kc, :], w=[("wo", kc)])
    wrf = A.alloc([128, 8, 36], F32)
    wrh = A.alloc([128, 8, 36], BF16)
    wrl = A.alloc([128, 8, 36], BF16)
    wrt = A.alloc([128, 8, 36], F32)
    P.dma("sync", wrf, w_r_d.rearrange("(kc p) c -> p kc c", p=128), w=["wrf"])
    I("dve", "tensor_copy", ["wrf"], ["wrh"], out=wrh, in_=wrf)
    I("dve", "tensor_tensor", ["wrf", "wrh"], ["wrt"], out=wrt, in0=wrf, in1=wrh, op=ALU.subtract)
    I("dve", "tensor_copy", ["wrt"], ["wrl"], out=wrl, in_=wrt)
    xts = [A.alloc([128, D], F32) for _ in range(3)]
    mTs = [A.alloc([128, 8, 128], BF16) for _ in range(2)]
    x1s = [A.alloc([128, D], F32) for _ in range(2)]
    hnf = A.alloc([128, D], F32)
    hnh = [A.alloc([128, D], BF16) for _ in range(2)]
    hnl = A.alloc([128, D], BF16)
    hiT = [A.alloc([128, 8, 128], BF16) for _ in range(2)]
    loT = A.alloc([128, 8, 128], BF16)
    lg = A.alloc([128, 36], F32)
    rt = A.alloc([128, 64], F32)

    def load_xo(bi):
        if bi >= NBLK:
            return
        sl = bi % 3
        P.dma("sync", xts[sl], x_own[bi * 128:(bi + 1) * 128, :], w=[("xt", sl)])

    load_xo(0)
    load_xo(1)
    for bi in range(NBLK):
        load_xo(bi + 2)
        sl = bi % 3
        ps = bi % 2
        transpose_block(mix[:, bi, :], ("mix", bi), 8, mTs[ps], ("mT", ps), ps)
        for half in range(2):
            for kc in range(8):
                mm(bank(2 + half), mTs[ps][:, kc, :], wo[:, kc, half * 512:(half + 1) * 512], kc == 0, kc == 7,
                   [("mT", ps), ("wo", kc)], [bk(2 + half)])
        x1 = x1s[ps]
        x1k = ("x1", ps)
        I("dve", "tensor_tensor", [bk(2), bk(3), ("xt", sl)], [x1k], out=x1, in0=psum[:, 1024:2048], in1=xts[sl], op=ALU.add)
        k = ("x1s", bi)
        scr_keys["x1"].append(k)
        P.dma("sync", x1_d[bi * 128:(bi + 1) * 128, :], x1, r=[x1k], w=[k])
        ssq, ssqk = sumsq(x1, x1k)
        rstd, rk = rstd_ops(ssq, ssqk, D)
        I("dve", "scalar_tensor_tensor", [x1k, rk, "bc_moe_norm"], ["hnf"], out=hnf, in0=x1, scalar=rstd, in1=bc["moe_norm"],
          op0=ALU.mult, op1=ALU.mult)
        hh = hnh[ps]
        I("pool", "tensor_copy", ["hnf"], [("hnh", ps)], out=hh, in_=hnf)
        I("pool", "tensor_tensor", ["hnf", ("hnh", ps)], ["hnl"], out=hnl, in0=hnf, in1=hh, op=ALU.subtract)
        transpose_block(hh, ("hnh", ps), 8, hiT[ps], ("hiT", ps), 4 + ps)
        k = ("hnTs", bi)
        scr_keys["hnT"].append(k)
        P.dma("sync", hnT_d[bi], hiT[ps].rearrange("p a b -> p (a b)"), r=[("hiT", ps)], w=[k])
        transpose_block(hnl, "hnl", 8, loT, "loT", 6, eng="dve")
        combos = [(hiT[ps], ("hiT", ps), wrh, "wrh"), (hiT[ps], ("hiT", ps), wrl, "wrl"), (loT, "loT", wrh, "wrh")]
        n = 0
        for (lt, ltk, wr, wrk) in combos:
            for kc in range(8):
                mm(bank(7)[:, 0:36], lt[:, kc, :], wr[:, kc, :], n == 0, n == 23, [ltk, wrk], [bk(7)])
                n += 1
        I("dve", "tensor_copy", [bk(7)], ["lg"], out=lg, in_=bank(7)[:, 0:36])
        gl = lg[:, 0:4]
        el = lg[:, 4:36].rearrange("p (g e) -> p g e", e=8)
        gmax, goh, ngmax, gexp, gsum, ggate = rt[:, 0:1], rt[:, 1:5], rt[:, 5:6], rt[:, 6:10], rt[:, 10:11], rt[:, 11:12]
        ing, m1, oh1, msk, m2, oh2 = rt[:, 12:20], rt[:, 20:21], rt[:, 21:29], rt[:, 29:37], rt[:, 37:38], rt[:, 38:46]
        dd, e2, den, w1, w2, g8 = rt[:, 46:47], rt[:, 47:48], rt[:, 48:49], rt[:, 49:50], rt[:, 50:51], rt[:, 51:59]
        R = ["lg", "rt"]
        W = ["rt"]

        def dv(meth, **kw):
            I("dve", meth, R, W, **kw)

        dv("reduce_max", out=gmax, in_=gl, axis=AX.X)
        dv("tensor_scalar", out=goh, in0=gl, scalar1=gmax, scalar2=None, op0=ALU.is_equal)
        dv("tensor_scalar", out=ngmax, in0=gmax, scalar1=-1.0, scalar2=None, op0=ALU.mult)
        I("pool", "memset", R, W, gsum, 0.0)
        I("act", "activation", R, W, out=gexp, in_=gl, func=AF.Exp, bias=ngmax, accum_out=gsum)
        dv("reciprocal", out=ggate, in_=gsum)
        dv("tensor_scalar", out=ing, in0=el[:, 0, :], scalar1=goh[:, 0:1], scalar2=None, op0=ALU.mult)
        for gi in range(1, 4):
            dv("scalar_tensor_tensor", out=ing, in0=el[:, gi, :], scalar=goh[:, gi:gi + 1], in1=ing, op0=ALU.mult, op1=ALU.add)
        dv("reduce_max", out=m1, in_=ing, axis=AX.X)
        dv("tensor_scalar", out=oh1, in0=ing, scalar1=m1, scalar2=None, op0=ALU.is_equal)
        dv("scalar_tensor_tensor", out=msk, in0=oh1, scalar=-1e30, in1=ing, op0=ALU.mult, op1=ALU.add)
        dv("reduce_max", out=m2, in_=msk, axis=AX.X)
        dv("tensor_scalar", out=oh2, in0=msk, scalar1=m2, scalar2=None, op0=ALU.is_equal)
        dv("tensor_tensor", out=dd, in0=m2, in1=m1, op=ALU.subtract)
        I("act", "activation", R, W, out=e2, in_=dd, func=AF.Exp)
        dv("tensor_scalar", out=den, in0=e2, scalar1=1.0, scalar2=None, op0=ALU.add)
        dv("reciprocal", out=den, in_=den)
        dv("tensor_tensor", out=w1, in0=den, in1=ggate, op=ALU.mult)
        dv("tensor_tensor", out=w2, in0=e2, in1=w1, op=ALU.mult)
        dv("tensor_scalar", out=g8, in0=oh1, scalar1=w1, scalar2=None, op0=ALU.mult)
        dv("scalar_tensor_tensor", out=g8, in0=oh2, scalar=w2, in1=g8, op0=ALU.mult, op1=ALU.add)
        for gi in range(4):
            I("dve", "tensor_scalar", R, [("gates", bi)], out=gates[:, bi, gi * 8:(gi + 1) * 8], in0=g8,
              scalar1=goh[:, gi:gi + 1], scalar2=None, op0=ALU.mult)
    A.pop()

    if "gates" in dbg_out:
        P.dma("sync", dbg_out["gates"].rearrange("(b p) e -> p b e", p=128), gates,
              r=[("gates", bi) for bi in range(NBLK)], w=["dbg_gates"])
        fin_keys.append("dbg_gates")
    if "x1" in dbg_out:
        P.dma("sync", dbg_out["x1"], x1_d, r=scr_keys["x1"], w=["dbg_x1"])
        fin_keys.append("dbg_x1")
    if stop_after == "D":
        return finish()
    A.pop()
    P.barrier()

    p_own = din("p_own", [NOWN, 256])
    w_eg = din("w_eg", [32, D, 512])
    w_eu = din("w_eu", [32, D, 512])
    w_ed = din("w_ed", [32, 512, D])
    w_pg = din("w_ple_gate", [D, D])
    w_pp = din("w_ple_proj", [256, D])
    A.push()
    BPP = NBLK // NSPLIT
    xres = A.alloc([128, BPP, D], F32)
    hnTp = A.alloc([128, BPP, D], BF16)
    wgs = [A.alloc([128, 8, 512], BF16) for _ in range(2)]
    wus = [A.alloc([128, 8, 512], BF16) for _ in range(2)]
    wds = [A.alloc([128, 4, D], BF16) for _ in range(2)]
    wpg = A.alloc([128, 8, D], BF16)
    wpp = A.alloc([128, 2, D], BF16)
    wpgv = w_pg.rearrange("(kc p) c -> p kc c", p=128)
    for kc in range(8):
        P.dma("pool", wpg[:, kc, :], wpgv[:, kc, :], w=[("wpg", kc)])
    P.dma("pool", wpp, w_pp.rearrange("(kc p) c -> p kc c", p=128), w=["wpp"])
    sgs = [A.alloc([128, 512], F32) for _ in range(2)]
    acts = [A.alloc([128, 512], BF16) for _ in range(2)]
    actTs = [A.alloc([128, 4, 128], BF16) for _ in range(2)]
    hp = A.alloc([128, D], BF16)
    hpT = A.alloc([128, 8, 128], BF16)
    pbs = [A.alloc([128, 256], BF16) for _ in range(2)]
    pT = A.alloc([128, 2, 128], BF16)
    glt = A.alloc([128, D], F32)
    gst = A.alloc([128, D], F32)
    x3 = A.alloc([128, D], F32)
    outs = [A.alloc([128, D], F32) for _ in range(2)]

    n_items = NSPLIT * 32

    def load_w(i):
        if i >= n_items:
            return
        ex = i % 32
        sl = i % 2
        P.dma("pool", wgs[sl], w_eg[ex].rearrange("(kc p) f -> p kc f", p=128), w=[("wg", sl)])
        P.dma("pool", wus[sl], w_eu[ex].rearrange("(kc p) f -> p kc f", p=128), w=[("wu", sl)])
        P.dma("pool", wds[sl], w_ed[ex].rearrange("(fc p) c -> p fc c", p=128), w=[("wd", sl)])

    load_w(0)
    cnt = 0
    for part in range(NSPLIT):
        b0 = part * BPP
        P.dma("sync", xres, x1_d[b0 * 128:(b0 + BPP) * 128, :].rearrange("(j p) c -> p j c", p=128),
              r=scr_keys["x1"], w=[("xres", j) for j in range(BPP)])
        P.dma("sync", hnTp, hnT_d[b0:b0 + BPP].rearrange("j p c -> p j c"), r=scr_keys["hnT"], w=["hnTp"])
        for ex in range(32):
            i = part * 32 + ex
            load_w(i + 1)
            sl = i % 2
            wg, wu, wd = wgs[sl], wus[sl], wds[sl]
            for j in range(BPP):
                bi = b0 + j
                q = cnt % 2
                cnt += 1
                bg, bu = 2 * q, 2 * q + 1
                for (bnk, wt, wtk) in ((bg, wg, ("wg", sl)), (bu, wu, ("wu", sl))):
                    for kc in range(8):
                        mm(bank(bnk), hnTp[:, j, kc * 128:(kc + 1) * 128], wt[:, kc, :], kc == 0, kc == 7,
                           ["hnTp", wtk], [bk(bnk)])
                I("act", "activation", [bk(bg)], [("sg", q)], out=sgs[q], in_=bank(bg), func=AF.Silu)
                I("dve", "scalar_tensor_tensor", [bk(bu), ("gates", bi), ("sg", q)], [("act", q)], out=acts[q], in0=bank(bu),
                  scalar=gates[:, bi, ex:ex + 1], in1=sgs[q], op0=ALU.mult, op1=ALU.mult)
                transpose_block(acts[q], ("act", q), 4, actTs[q], ("actT", q), 4)
                for half in range(2):
                    for fc in range(4):
                        mm(bank(5 + half), actTs[q][:, fc, :], wd[:, fc, half * 512:(half + 1) * 512], fc == 0, fc == 3,
                           [("actT", q), ("wd", sl)], [bk(5 + half)])
                I("dve", "tensor_tensor", [bk(5), bk(6), ("xres", j)], [("xres", j)], out=xres[:, j, :],
                  in0=psum[:, 5 * 512:7 * 512], in1=xres[:, j, :], op=ALU.add)
        for j in range(BPP):
            bi = b0 + j
            x2 = xres[:, j, :]
            x2k = ("xres", j)
            pb = pbs[j % 2]
            P.dma("pool", pb, p_own[bi * 128:(bi + 1) * 128, :], w=[("pb", j % 2)])
            ssq, ssqk = sumsq(x2, x2k)
            rstd, rk = rstd_ops(ssq, ssqk, D)
            I("dve", "scalar_tensor_tensor", [x2k, rk, "bc_ple_norm"], ["hp"], out=hp, in0=x2, scalar=rstd, in1=bc["ple_norm"],
              op0=ALU.mult, op1=ALU.mult)
            transpose_block(hp, "hp", 8, hpT, "hpT", 7)
            for half in range(2):
                for kc in range(8):
                    mm(bank(half), hpT[:, kc, :], wpg[:, kc, half * 512:(half + 1) * 512], kc == 0, kc == 7,
                       ["hpT", ("wpg", kc)], [bk(half)])
            transpose_block(pb, ("pb", j % 2), 2, pT, "pT", 4)
            for half in range(2):
                for kc in range(2):
                    mm(bank(2 + half), pT[:, kc, :], wpp[:, kc, half * 512:(half + 1) * 512], kc == 0, kc == 1,
                       ["pT", "wpp"], [bk(2 + half)])
            I("dve", "tensor_tensor", [bk(0), bk(1), "bc_b_ple_gate"], ["glt"], out=glt, in0=psum[:, 0:1024],
              in1=bc["b_ple_gate"], op=ALU.add)
            I("act", "activation", ["glt"], ["gst"], out=gst, in_=glt, func=AF.Sigmoid)
            I("dve", "tensor_tensor", ["gst", bk(2), bk(3)], ["gst"], out=gst, in0=gst, in1=psum[:, 1024:2048], op=ALU.mult)
            I("pool", "tensor_tensor", ["gst", x2k], ["x3"], out=x3, in0=gst, in1=x2, op=ALU.add)
            ssq3, ssq3k = sumsq(x3, "x3")
            rstd3, r3k = rstd_ops(ssq3, ssq3k, D)
            ot = outs[j % 2]
            I("dve", "scalar_tensor_tensor", ["x3", r3k, "bc_final_norm"], [("ot", j % 2)], out=ot, in0=x3, scalar=rstd3,
              in1=bc["final_norm"], op0=ALU.mult, op1=ALU.mult)
            P.dma("sync", out_d[bi * 128:(bi + 1) * 128, :], ot, r=[("ot", j % 2)], w=[("out", bi)])
            fin_keys.append(("out", bi))
    A.pop()
    return finish()


def _rope_tables():
    inv = (1.0 / (np.float32(10000.0) ** (np.arange(0, 64, 2, dtype=np.float32) / np.float32(64)))).astype(np.float32)
    ang = np.arange(S, dtype=np.float32)[:, None] * inv[None, :]
    ang = np.concatenate([ang, ang], axis=-1)
    cos = np.cos(ang).astype(np.float32)
    sin = np.sin(ang).astype(np.float32)
    sgn = np.concatenate([-np.ones(32, np.float32), np.ones(32, np.float32)])
    cosT = np.concatenate([cos.T, cos.T], axis=0)
    sinT = np.concatenate([(sin * sgn).T, (sin * sgn).T], axis=0)
    return np.ascontiguousarray(np.stack([cosT, sinT]))


_NC_CACHE = {}


def _prepare(inputs):
    inp = {k: np.asarray(v) for k, v in inputs.items()}
    x = inp["x"]
    p = inp["p"][0]
    w_in = inp["w_in"][0]
    idx = np.arange(512).reshape(4, 2, 64)
    rot_idx = np.concatenate([idx[..., 32:], idx[..., :32]], axis=-1).reshape(-1)
    wq_c, wk_c, wv_c, wg_c = w_in[:, 0:512], w_in[:, 512:1024], w_in[:, 1024:1536], w_in[:, 1536:2560]
    w_kv = np.ascontiguousarray(np.concatenate([wk_c, wk_c[:, rot_idx], wv_c], axis=1))
    w_q = np.ascontiguousarray(np.concatenate([wq_c, wq_c[:, rot_idx], wg_c], axis=1))
    cs = _rope_tables()
    tri = (np.arange(128)[:, None] <= np.arange(128)[None, :]).astype(np.float32)
    ones = np.ones((128, 128), np.float32)
    zeros = np.zeros((128, 128), np.float32)
    shared = {
        "w_kv": w_kv, "w_q": w_q, "ident": np.eye(128, dtype=np.float32),
        "wsT": np.ascontiguousarray(inp["gm_spatial_w"][0].transpose(0, 2, 1)),
        "bsT": np.ascontiguousarray(inp["gm_spatial_b"][0].T),
        "w_out": inp["w_out"][0],
        "w_router": np.ascontiguousarray(np.concatenate([inp["w_group_router"][0], inp["w_expert_router"][0]], axis=1)),
        "w_eg": inp["w_expert_gate"][0], "w_eu": inp["w_expert_up"][0], "w_ed": inp["w_expert_down"][0],
        "w_ple_gate": inp["w_ple_gate"][0], "w_ple_proj": inp["w_ple_proj"][0],
        "final_norm": inp["final_norm"].reshape(1, -1),
    }
    for n in ["attn_norm", "moe_norm", "ple_norm", "b_ple_gate", "gm_ln_gain", "gm_ln_bias", "gm_out_norm",
              "diff_subln", "lambda_q1", "lambda_k1", "lambda_q2", "lambda_k2"]:
        shared[n] = inp[n][0].reshape(1, -1)
    in_maps = []
    for c in range(8):
        b, s = c // 2, c % 2
        xb = x[b].reshape(64, 128, D)
        pos = (np.arange(s, 64, 2)[:, None] * 128 + np.arange(128)[None, :]).reshape(-1)
        m = dict(shared)
        m["x_full"] = np.ascontiguousarray(x[b])
        m["x_own"] = np.ascontiguousarray(xb[s::2].reshape(NOWN, D))
        m["p_own"] = np.ascontiguousarray(p[b].reshape(64, 128, 256)[s::2].reshape(NOWN, 256))
        m["cs_full"] = cs
        m["cs_own"] = np.ascontiguousarray(cs[:, :, pos])
        m["masks"] = np.ascontiguousarray(np.stack([tri if s == 0 else ones, zeros if s == 0 else tri, tri]))
        in_maps.append(m)
    return in_maps


def kernel(**inputs):
    in_maps = _prepare(inputs)
    if "nc" not in _NC_CACHE:
        _NC_CACHE["nc"] = build_program()
    nc = _NC_CACHE["nc"]
    res = run_bass_kernel_spmd(nc, in_maps, core_ids=list(range(8)))
    out = np.empty((4, S, D), np.float32)
    for c in range(8):
        b, s = c // 2, c % 2
        out[b].reshape(64, 128, D)[s::2] = res.results[c]["out"].reshape(32, 128, D)
    return out
```

```python
import contextlib
import numpy as np
import concourse.bass as bass
import concourse.mybir as mybir
from concourse.bass_utils import run_bass_kernel_spmd

F32 = mybir.dt.float32
BF16 = mybir.dt.bfloat16
AF = mybir.ActivationFunctionType
ALU = mybir.AluOpType
AX = mybir.AxisListType

D = 1024
S = 8192
NOWN = 4096
NBLK = 32
EPS = 1e-6
ENGS = ["sync", "act", "dve", "pool", "pe"]
NPOOL = 8
CAP = 512
CBLK = CAP // 128
NSLOT = 32 * CAP
I32 = mybir.dt.int32
LAMBDA_INIT = 0.2


class Op:
    __slots__ = ("eng", "fn", "deps", "dma", "needs_sig", "sigval", "pool", "val", "prev_val")

    def __init__(self, eng, fn, dma):
        self.eng = eng
        self.fn = fn
        self.dma = dma
        self.deps = ()
        self.needs_sig = False
        self.sigval = 0
        self.pool = 0
        self.val = 0
        self.prev_val = 0


class Prog:
    def __init__(self, nc):
        self.nc = nc
        self.ops = {e: [] for e in ENGS}
        self.lastw = {}
        self.readers = {}
        self.dma_uses = {e: [0] * NPOOL for e in ENGS}
        self.dma_rr = {e: 0 for e in ENGS}

    def add(self, eng, fn, r=(), w=(), dma=False):
        op = Op(eng, fn, dma)
        deps = set()
        for k in r:
            lw = self.lastw.get(k)
            if lw is not None:
                deps.add(lw)
        for k in w:
            lw = self.lastw.get(k)
            if lw is not None:
                deps.add(lw)
            for rd in self.readers.get(k, ()):
                deps.add(rd)
        deps.discard(op)
        fl = []
        for d in deps:
            if (not d.dma) and (not dma) and d.eng == eng == "pe":
                continue
            fl.append(d)
            if not d.dma:
                d.needs_sig = True
        op.deps = fl
        for k in r:
            self.readers.setdefault(k, []).append(op)
        for k in w:
            self.lastw[k] = op
            self.readers[k] = []
        if dma:
            i = self.dma_rr[eng]
            self.dma_rr[eng] = (i + 1) % NPOOL
            op.pool = i
            op.prev_val = 16 * self.dma_uses[eng][i]
            self.dma_uses[eng][i] += 1
            op.val = 16 * self.dma_uses[eng][i]
        self.ops[eng].append(op)
        return op

    def I(self, eng, meth, r, w, *args, **kw):
        def fn(e):
            return getattr(e, meth)(*args, **kw)
        return self.add(eng, fn, r, w)

    def dma(self, eng, out, in_, r=(), w=()):
        def fn(e):
            return e.dma_start(out=out, in_=in_)
        return self.add(eng, fn, r, w, dma=True)

    def dmafn(self, eng, fn, r=(), w=()):
        return self.add(eng, fn, r, w, dma=True)

    def wait(self, eng, r):
        return self.add(eng, None, r, ())

    def barrier(self):
        deps = []
        for e in ENGS:
            seen_c = False
            for op in reversed(self.ops[e]):
                if op.fn is None:
                    if op.eng == "__bar__":
                        break
                    continue
                if op.dma:
                    deps.append(op)
                elif not seen_c:
                    seen_c = True
                    op.needs_sig = True
                    deps.append(op)
        for e in ENGS:
            op = Op(e, None, False)
            op.deps = list(deps)
            self.ops[e].append(op)

    def emit(self):
        nc = self.nc
        for e in ENGS:
            cnt = 0
            for op in self.ops[e]:
                if op.dma or op.fn is None:
                    continue
                if op.needs_sig:
                    cnt += 1
                    op.sigval = cnt
        with contextlib.ExitStack() as st:
            csem = {e: st.enter_context(nc.semaphore("c_" + e)) for e in ENGS}
            dsem = {}
            for e in ENGS:
                if any(o.dma for o in self.ops[e]):
                    for i in range(NPOOL):
                        dsem[(e, i)] = st.enter_context(nc.semaphore("d_%s_%d" % (e, i)))
            block = st.enter_context(nc.Block())

            def run(e, eh):
                waited = {}
                for op in self.ops[e]:
                    need = {}
                    for d in op.deps:
                        if d.dma:
                            key = (d.eng, d.pool)
                            val = d.val
                        else:
                            key = d.eng
                            val = d.sigval
                        if need.get(key, 0) < val:
                            need[key] = val
                    if op.dma and op.prev_val > 0:
                        key = (e, op.pool)
                        if need.get(key, 0) < op.prev_val:
                            need[key] = op.prev_val
                    for key, val in need.items():
                        if waited.get(key, 0) < val:
                            sem = dsem[key] if isinstance(key, tuple) else csem[key]
                            eh.wait_ge(sem, val)
                            waited[key] = val
                    if op.fn is None:
                        continue
                    ins = op.fn(eh)
                    if op.dma:
                        ins.then_inc(dsem[(e, op.pool)], 16)
                    elif op.needs_sig:
                        ins.then_inc(csem[e], 1)

            block.sync(lambda eh: run("sync", eh))
            block.scalar(lambda eh: run("act", eh))
            block.vector(lambda eh: run("dve", eh))
            block.gpsimd(lambda eh: run("pool", eh))
            block.tensor(lambda eh: run("pe", eh))


class Arena:
    def __init__(self, nc, nbytes):
        self.t = nc.alloc_sbuf_tensor("arena", [128, nbytes // 4], F32)
        self.nbytes = nbytes
        self.off = 0
        self.marks = []

    def push(self):
        self.marks.append(self.off)

    def pop(self):
        self.off = self.marks.pop()

    def alloc(self, shape, dtype):
        esz = 2 if dtype == BF16 else 4
        n = 1
        for s in shape[1:]:
            n *= s
        nb = (n * esz + 31) // 32 * 32
        assert self.off + nb <= self.nbytes, ("sbuf arena overflow", self.off, nb)
        ap = self.t[:, self.off // 4:(self.off + nb) // 4]
        if dtype != F32:
            ap = ap.bitcast(dtype)
        ap = ap[0:shape[0], 0:n]
        self.off += nb
        if len(shape) == 3:
            ap = ap.rearrange("p (a b) -> p a b", b=shape[2])
        elif len(shape) == 4:
            ap = ap.rearrange("p (a b c) -> p a b c", b=shape[2], c=shape[3])
        return ap


def build_program(debug=None):
    nc = bass.Bass("TRN2", target_bir_lowering=False)
    P = Prog(nc)
    I = P.I

    def din(name, shape, dt=F32):
        return nc.dram_tensor(name, list(shape), dt, kind="ExternalInput").ap()

    x_full = din("x_full", [S, D])
    x_own = din("x_own", [NOWN, D])
    w_kv = din("w_kv", [D, 1536])
    w_q = din("w_q", [D, 2048])
    cs_full = din("cs_full", [2, 128, S])
    cs_own = din("cs_own", [2, 128, NOWN])
    masks_d = din("masks", [3, 128, 128])
    ident_d = din("ident", [128, 128])
    wsT_d = din("wsT", [4, 128, 128])
    bsT_d = din("bsT", [128, 4])
    vec_d = {n: din(n, [1, sz]) for n, sz in [
        ("attn_norm", D), ("moe_norm", D), ("ple_norm", D), ("final_norm", D), ("b_ple_gate", D),
        ("gm_ln_gain", 512), ("gm_ln_bias", 512), ("gm_out_norm", 512), ("diff_subln", 128),
        ("lambda_q1", 64), ("lambda_k1", 64), ("lambda_q2", 64), ("lambda_k2", 64)]}
    w_out_d = din("w_out", [D, D])
    w_r_d = din("w_router", [D, 36])
    out_d = nc.dram_tensor("out", [NOWN, D], F32, kind="ExternalOutput").ap()

    kt_d = nc.dram_tensor("kt_scr", [128, 4, S], BF16, kind="Internal").ap()
    v_d = nc.dram_tensor("v_scr", [4, 128, 64 * 130], BF16, kind="Internal").ap()
    qt_d = nc.dram_tensor("qt_scr", [128, 4, NOWN], BF16, kind="Internal").ap()
    x1_d = nc.dram_tensor("x1_scr", [NOWN, D], F32, kind="Internal").ap()
    xbuf_d = nc.dram_tensor("xbuf_scr", [NSLOT + 128, D], BF16, kind="Internal").ap()
    ybuf_d = nc.dram_tensor("ybuf_scr", [NSLOT + 128, D], F32, kind="Internal").ap()
    ebase_d = din("ebase", [1, 32])
    dbg_out = {}
    if debug:
        for n, shp in debug.items():
            if n.startswith("_"):
                continue
            dbg_out[n] = nc.dram_tensor("dbg_" + n, list(shp), F32, kind="ExternalOutput").ap()
    stop_after = (debug or {}).get("_stop", None)
    fin_keys = []

    A = Arena(nc, 206 * 1024)
    psum = nc.alloc_psum_tensor("psum", [128, 4096], F32)

    def bank(b, n=512):
        return psum[:, b * 512:b * 512 + n]

    def bank_bf(b):
        return psum[:, b * 512:(b + 1) * 512].bitcast(BF16)

    def bk(b):
        return ("bank", b)

    ident = A.alloc([128, 128], BF16)
    P.dma("pool", ident, ident_d, w=["ident"])
    bc = {}
    for n, ap in vec_d.items():
        sz = ap.shape[1]
        t = A.alloc([128, sz], F32)
        bc[n] = t
        P.dma("sync", t, ap.broadcast_to([128, sz]), w=["bc_" + n])
    gates = A.alloc([128, NBLK, 32], F32)
    slots_i = A.alloc([128, NBLK, 2], I32)
    gw = A.alloc([128, NBLK, 2], F32)
    stat = A.alloc([128, 256], F32)
    stat_i = [0]

    def st1(n=1):
        i = stat_i[0]
        if i + n > 256:
            i = 0
        stat_i[0] = i + n
        return stat[:, i:i + n], ("stat", i)

    junk = A.alloc([128, 1024], BF16)

    lam_t = A.alloc([128, 8], F32)
    lj = A.alloc([128, 64], F32)
    I("pool", "memset", [], ["lam"], lam_t, 0.0)
    for i, (a, b) in enumerate([("lambda_q1", "lambda_k1"), ("lambda_q2", "lambda_k2")]):
        I("dve", "tensor_tensor", ["bc_" + a, "bc_" + b], ["lj"], out=lj, in0=bc[a], in1=bc[b], op=ALU.mult)
        I("dve", "reduce_sum", ["lj", "lam"], ["lam"], out=lam_t[:, i:i + 1], in_=lj, axis=AX.X)
    I("act", "activation", ["lam"], ["lam"], out=lam_t[:, 2:4], in_=lam_t[:, 0:2], func=AF.Exp)
    I("dve", "tensor_tensor", ["lam"], ["lam"], out=lam_t[:, 4:5], in0=lam_t[:, 3:4], in1=lam_t[:, 2:3], op=ALU.subtract)
    I("dve", "tensor_scalar", ["lam"], ["lam"], out=lam_t[:, 5:6], in0=lam_t[:, 4:5], scalar1=-LAMBDA_INIT, scalar2=None,
      op0=ALU.add)
    neglam = lam_t[:, 5:6]
    I("dve", "tensor_scalar", ["bc_diff_subln"], ["bc_diff_subln"], out=bc["diff_subln"], in0=bc["diff_subln"],
      scalar1=1.0 - LAMBDA_INIT, scalar2=None, op0=ALU.mult)

    def rstd_ops(ssq, ssq_k, n, eps=EPS):
        t1, k1 = st1()
        t2, k2 = st1()
        t3, k3 = st1()
        I("dve", "tensor_scalar", [ssq_k], [k1], out=t1, in0=ssq, scalar1=1.0 / n, scalar2=eps, op0=ALU.mult, op1=ALU.add)
        I("act", "sqrt", [k1], [k2], out=t2, in_=t1)
        I("dve", "reciprocal", [k2], [k3], out=t3, in_=t2)
        return t3, k3

    def sumsq(src, src_k, bias=None, bias_k=None):
        s, k = st1()
        I("pool", "memset", [], [k], s, 0.0)
        n = src.shape[1]
        if bias is None:
            I("act", "activation", [src_k, k], [k, "junk"], out=junk[:, 0:n], in_=src, func=AF.Square, accum_out=s)
        else:
            I("act", "activation", [src_k, k, bias_k], [k, "junk"], out=junk[:, 0:n], in_=src, func=AF.Square, bias=bias,
              accum_out=s)
        return s, k

    def transpose_block(src, src_k, nch, dst, dst_k, tb, eng="act"):
        pt = bank_bf(tb)
        for c in range(nch):
            I("pe", "transpose", [src_k, "ident"], [bk(tb)], out=pt[:, c * 128:(c + 1) * 128], in_=src[:, c * 128:(c + 1) * 128],
              identity=ident)
        pv = pt[:, 0:nch * 128].rearrange("p (a b) -> p a b", b=128)
        if eng == "act":
            I("act", "copy", [bk(tb)], [dst_k], out=dst, in_=pv)
        else:
            I("dve", "tensor_copy", [bk(tb)], [dst_k], out=dst, in_=pv)

    def norm_transpose(xt, xt_k, gain, gain_k, hT_dst, hT_k, tb, hb, hb_k):
        ssq, ssq_k = sumsq(xt, xt_k)
        rstd, rk = rstd_ops(ssq, ssq_k, D)
        I("dve", "scalar_tensor_tensor", [xt_k, rk, gain_k], [hb_k], out=hb, in0=xt, scalar=rstd, in1=gain,
          op0=ALU.mult, op1=ALU.mult)
        transpose_block(hb, hb_k, 8, hT_dst, hT_k, tb)

    def mm(out, lhsT, rhs, start, stop, r, w, skip=False):
        if skip:
            I("pe", "matmul", r, w, out, lhsT=lhsT, rhs=rhs, start=start, stop=stop, skip_group_check=True)
        else:
            I("pe", "matmul", r, w, out, lhsT=lhsT, rhs=rhs, start=start, stop=stop)

    scr_keys = {"kt": [], "qt": [], "v": [], "x1": [], "hnT": [], "xbuf": [], "ybuf": []}

    def proj_phase(which):
        A.push()
        is_a = which == "A"
        ncols = 1536 if is_a else 2048
        wsrc = w_kv if is_a else w_q
        xsrc = x_full if is_a else x_own
        cs = cs_full if is_a else cs_own
        ngrp = 16 if is_a else 8
        dst_d = kt_d if is_a else qt_d
        skey = "kt" if is_a else "qt"
        wb = A.alloc([128, 8, ncols], BF16)
        wv = wsrc.rearrange("(kc p) c -> p kc c", p=128)
        for kc in range(8):
            P.dma("pool", wb[:, kc, :], wv[:, kc, :], w=[("wproj", kc)])
        xts = [A.alloc([128, D], F32) for _ in range(3)]
        hbs = [A.alloc([128, D], BF16) for _ in range(2)]
        hTs = [A.alloc([128, 8, 512], BF16) for _ in range(2)]
        cst = [A.alloc([128, 2, 512], F32) for _ in range(2)]
        t1s = [A.alloc([128, 512], F32) for _ in range(2)]
        t2s = [A.alloc([128, 512], F32) for _ in range(2)]
        stg = [A.alloc([128, 4, 512], BF16) for _ in range(2)]
        if is_a:
            vgs = [A.alloc([128, 4, 4, 130], BF16) for _ in range(2)]
            for i in range(2):
                I("pool", "memset", [], [("vg", i)], vgs[i], 1.0)
        else:
            wsT = A.alloc([128, 4, 128], BF16)
            wsf = A.alloc([128, 4, 128], F32)
            tri = A.alloc([128, 128], F32)
            bsT = A.alloc([128, 4], F32)
            P.dma("sync", wsf, wsT_d.rearrange("h s t -> s h t"), w=["wsf"])
            P.dma("sync", tri, masks_d[2], w=["tri"])
            P.dma("sync", bsT, bsT_d, w=["bsT"])
            for h in range(4):
                I("dve", "tensor_tensor", ["wsf", "tri"], ["wsT"], out=wsT[:, h, :], in0=wsf[:, h, :], in1=tri, op=ALU.mult)
            bufA = A.alloc([128, 1024], F32)
            bufB = A.alloc([128, 1024], F32)
            vn1 = A.alloc([128, 512], F32)
            vnb = A.alloc([128, 512], BF16)

        nblk = ngrp * 4

        def load_x(bi):
            if bi >= nblk:
                return
            sl = bi % 3
            P.dma("sync", xts[sl], xsrc[bi * 128:(bi + 1) * 128, :], w=[("xt", sl)])

        def load_cs(g):
            if g >= ngrp:
                return
            sl = g % 2
            P.dma("sync", cst[sl], cs[:, :, g * 512:(g + 1) * 512].rearrange("a p t -> p a t"), w=[("cs", sl)])

        load_x(0)
        load_x(1)
        load_cs(0)
        for g in range(ngrp):
            gs = g % 2
            hT = hTs[gs]
            load_cs(g + 1)
            for j in range(4):
                bi = g * 4 + j
                load_x(bi + 2)
                sl = bi % 3
                norm_transpose(xts[sl], ("xt", sl), bc["attn_norm"], "bc_attn_norm",
                               hT[:, :, j * 128:(j + 1) * 128], ("hT", gs, j), bi % 2, hbs[bi % 2], ("hb", bi % 2))
            hkeys = [("hT", gs, j) for j in range(4)]
            for h in range(4):
                pb = 2 + 2 * (h % 2)
                for which_c, b in ((0, pb), (1, pb + 1)):
                    c0 = which_c * 512 + h * 128
                    for kc in range(8):
                        mm(bank(b), wb[:, kc, c0:c0 + 128], hT[:, kc, :], kc == 0, kc == 7, hkeys + [("wproj", kc)], [bk(b)])
                ts = h % 2
                I("dve", "tensor_tensor", [bk(pb), ("cs", gs)], [("t1", ts)], out=t1s[ts], in0=bank(pb), in1=cst[gs][:, 0, :],
                  op=ALU.mult)
                I("dve", "tensor_tensor", [bk(pb + 1), ("cs", gs)], [("t2", ts)], out=t2s[ts], in0=bank(pb + 1),
                  in1=cst[gs][:, 1, :], op=ALU.mult)
                I("pool", "tensor_tensor", [("t1", ts), ("t2", ts)], [("stg", gs)], out=stg[gs][:, h, :], in0=t1s[ts],
                  in1=t2s[ts], op=ALU.add)
            k = (skey, g)
            scr_keys[skey].append(k)
            P.dma("sync", dst_d[:, :, g * 512:(g + 1) * 512], stg[gs], r=[("stg", gs)], w=[k])
            if is_a:
                for j in range(4):
                    b = 6 + (j % 2)
                    for kc in range(8):
                        mm(bank(b), hT[:, kc, j * 128:(j + 1) * 128], wb[:, kc, 1024:1536], kc == 0, kc == 7,
                           [("hT", gs, j), ("wproj", kc)], [bk(b)])
                    I("act", "copy", [bk(b)], [("vg", gs)], out=vgs[gs][:, j, :, 0:128],
                      in_=bank(b).rearrange("p (h d) -> p h d", d=128))
                for h in range(4):
                    k = ("v", g, h)
                    scr_keys["v"].append(k)
                    P.dma("sync", v_d[h][:, g * 520:(g + 1) * 520].rearrange("p (j c) -> p j c", c=130), vgs[gs][:, :, h, :],
                          r=[("vg", gs)], w=[k])
            else:
                for j in range(4):
                    bi = g * 4 + j
                    for half in range(2):
                        for kc in range(8):
                            mm(bank(6 + half), hT[:, kc, j * 128:(j + 1) * 128],
                               wb[:, kc, 1024 + half * 512:1536 + half * 512], kc == 0, kc == 7,
                               [("hT", gs, j), ("wproj", kc)], [bk(6 + half)])
                    zp = psum[:, 6 * 512:8 * 512]
                    zk = [bk(6), bk(7)]
                    I("act", "activation", zk + ["bufA"], ["bufA"], out=bufA, in_=zp, func=AF.Square)
                    I("dve", "tensor_scalar", ["bufA"], ["bufA"], out=bufA, in0=bufA, scalar1=0.044715, scalar2=1.0,
                      op0=ALU.mult, op1=ALU.add)
                    I("dve", "tensor_tensor", ["bufA"] + zk, ["bufA"], out=bufA, in0=bufA, in1=zp, op=ALU.mult)
                    I("act", "activation", ["bufA", "bufB"], ["bufB"], out=bufB, in_=bufA, func=AF.Sigmoid,
                      scale=1.5957691216057308)
                    I("dve", "tensor_tensor", ["bufB", "bufA"] + zk, ["bufA"], out=bufA, in0=bufB, in1=zp, op=ALU.mult)
                    ga = bufA
                    u = ga[:, 0:512]
                    vv = ga[:, 512:1024]
                    og = bufB[:, 0:512]
                    s1, s1k = st1()
                    I("dve", "reduce_sum", ["bufA"], [s1k], out=s1, in_=vv, axis=AX.X)
                    nm, nmk = st1()
                    I("dve", "tensor_scalar", [s1k], [nmk], out=nm, in0=s1, scalar1=-1.0 / 512, scalar2=None, op0=ALU.mult)
                    ssq, ssqk = sumsq(vv, "bufA", bias=nm, bias_k=nmk)
                    rstd, rk = rstd_ops(ssq, ssqk, 512)
                    I("dve", "tensor_scalar", ["bufA", nmk, rk], ["vn1"], out=vn1, in0=vv, scalar1=nm, scalar2=rstd,
                      op0=ALU.add, op1=ALU.mult)
                    I("pool", "tensor_tensor", ["vn1", "bc_gm_ln_gain"], ["vn1"], out=vn1, in0=vn1, in1=bc["gm_ln_gain"],
                      op=ALU.mult)
                    I("pool", "tensor_tensor", ["vn1", "bc_gm_ln_bias"], ["vnb"], out=vnb, in0=vn1, in1=bc["gm_ln_bias"],
                      op=ALU.add)
                    for h in range(4):
                        mm(bank(2)[:, h * 128:(h + 1) * 128], wsT[:, h, :], vnb[:, h * 128:(h + 1) * 128], True, True,
                           ["wsT", "vnb"], [bk(2)])
                    for h in range(4):
                        I("dve", "scalar_tensor_tensor", [bk(2), "bsT", "bufA", "bufB"], ["bufB"], out=og[:, h * 128:(h + 1) * 128],
                          in0=bank(2)[:, h * 128:(h + 1) * 128], scalar=bsT[:, h:h + 1], in1=u[:, h * 128:(h + 1) * 128],
                          op0=ALU.add, op1=ALU.mult)
                    ssq2, ssq2k = sumsq(og, "bufB")
                    rstd2, r2k = rstd_ops(ssq2, ssq2k, 512)
                    I("dve", "scalar_tensor_tensor", ["bufB", r2k, "bc_gm_out_norm"], [("mix", bi)], out=mix[:, bi, 512:1024],
                      in0=og, scalar=rstd2, in1=bc["gm_out_norm"], op0=ALU.mult, op1=ALU.mult)
        A.pop()

    def dump_mix():
        if "mix" in dbg_out:
            P.barrier()
            A.push()
            mf = A.alloc([128, D], F32)
            for bi in range(NBLK):
                I("dve", "tensor_copy", [("mix", bi)], ["mf"], out=mf, in_=mix[:, bi, :])
                P.dma("sync", dbg_out["mix"][bi * 128:(bi + 1) * 128, :], mf, r=["mf"], w=[("dbgmix", bi)])
                fin_keys.append(("dbgmix", bi))
            A.pop()

    def finish():
        P.wait("sync", fin_keys)
        P.emit()
        return nc

    proj_phase("A")
    P.barrier()
    A.push()
    mix = A.alloc([128, NBLK, D], BF16)
    proj_phase("B")
    if stop_after == "B":
        dump_mix()
        return finish()

    P.barrier()
    A.push()
    KT = [A.alloc([128, S], BF16) for _ in range(2)]
    VH = [A.alloc([128, 64, 130], BF16) for _ in range(2)]
    QT = [A.alloc([128, NOWN], BF16) for _ in range(2)]
    PT = [[A.alloc([128, 512], BF16) for _ in range(3)] for _ in range(2)]
    mk = A.alloc([128, 2, 128], BF16)
    P.dma("pool", mk, masks_d[0:2].rearrange("a k q -> k a q"), w=["mk"])
    t1c = A.alloc([128, 128], F32)
    odc = A.alloc([128, 128], F32)

    def load_head(h):
        if h >= 4:
            return
        sl = h % 2
        P.dma("sync", KT[sl], kt_d[:, h, :], r=scr_keys["kt"], w=[("KT", sl)])
        P.dma("sync", VH[sl], v_d[h].rearrange("p (k c) -> p k c", c=130), r=scr_keys["v"], w=[("VH", sl)])
        P.dma("sync", QT[sl], qt_d[:, h, :], r=scr_keys["qt"], w=[("QT", sl)])

    def oreg(c, m):
        idx = c * 2 + m
        b = 4 + idx // 3
        off = (idx % 3) * 130
        return psum[:, b * 512 + off:b * 512 + off + 130], ("oacc", idx)

    load_head(0)
    for h in range(4):
        load_head(h + 1)
        hs = h % 2
        kt, vh, qt = KT[hs], VH[hs], QT[hs]
        hk = [("KT", hs), ("QT", hs)]
        for g in range(8):
            nkb = 8 * g + 8
            steps = [(kb, m) for kb in range(nkb) for m in range(2)]

            def cmin_of(kb):
                j = kb - (nkb - 8)
                return (j // 2) if j > 0 else 0

            def qk(si):
                kb, m = steps[si]
                cmin = cmin_of(kb)
                b = si % 4
                q0 = g * 512 + cmin * 128
                mm(bank(b)[:, cmin * 128:512], kt[m * 64:(m + 1) * 64, kb * 128:(kb + 1) * 128],
                   qt[m * 64:(m + 1) * 64, q0:(g + 1) * 512], True, True, hk, [bk(b)])

            qk(0)
            qk(1)
            for si in range(len(steps)):
                kb, m = steps[si]
                if si + 2 < len(steps):
                    qk(si + 2)
                j = kb - (nkb - 8)
                cmin = cmin_of(kb)
                b = si % 4
                pslot = (si // 2) % 3
                pt = PT[m][pslot]
                ptk = ("PT", m, pslot)
                I("act", "activation", [bk(b)], [ptk], out=pt[:, cmin * 128:512], in_=bank(b)[:, cmin * 128:512],
                  func=AF.Exp, scale=0.125)
                for c in range(cmin, 4):
                    if j == 2 * c or j == 2 * c + 1:
                        mi = j - 2 * c
                        I("pool", "tensor_tensor", [ptk, "mk"], [ptk], out=pt[:, c * 128:(c + 1) * 128],
                          in0=pt[:, c * 128:(c + 1) * 128], in1=mk[:, mi, :], op=ALU.mult)
                for c in range(cmin, 4):
                    oa, ok = oreg(c, m)
                    last = (nkb - 8) + 2 * c + 1
                    first_in_bank = (kb == 0 and m == 0 and c in (0, 2, 3))
                    mm(oa[:, 0:129], pt[:, c * 128:(c + 1) * 128], vh[:, kb, 0:129], first_in_bank, kb == last,
                       [ptk, ("VH", hs)], [ok], skip=True)
            for c in range(4):
                bi = g * 4 + c
                o1, k1 = oreg(c, 0)
                o2, k2 = oreg(c, 1)
                ri1, ri1k = st1()
                ri2, ri2k = st1()
                I("dve", "reciprocal", [k1], [ri1k], out=ri1, in_=o1[:, 128:129])
                I("dve", "reciprocal", [k2], [ri2k], out=ri2, in_=o2[:, 128:129])
                nl, nlk = st1()
                I("dve", "tensor_scalar", [ri2k, "lam"], [nlk], out=nl, in0=ri2, scalar1=neglam, scalar2=None, op0=ALU.mult)
                I("dve", "tensor_scalar", [k1, ri1k], ["t1c"], out=t1c, in0=o1[:, 0:128], scalar1=ri1, scalar2=None, op0=ALU.mult)
                I("dve", "scalar_tensor_tensor", [k2, nlk, "t1c"], ["odc"], out=odc, in0=o2[:, 0:128], scalar=nl, in1=t1c,
                  op0=ALU.mult, op1=ALU.add)
                ssq, ssqk = sumsq(odc, "odc")
                rstd, rk = rstd_ops(ssq, ssqk, 128)
                I("dve", "scalar_tensor_tensor", ["odc", rk, "bc_diff_subln"], [("mix", bi)],
                  out=mix[:, bi, h * 128:(h + 1) * 128], in0=odc, scalar=rstd, in1=bc["diff_subln"], op0=ALU.mult, op1=ALU.mult)
    A.pop()
    dump_mix()
    if stop_after == "C":
        return finish()

    P.barrier()
    A.push()
    wo = A.alloc([128, 8, D], BF16)
    wov = w_out_d.rearrange("(kc p) c -> p kc c", p=128)
    for kc in range(8):
        P.dma("pool", wo[:, kc, :], wov[:, kc, :], w=[("wo", kc)])
    wrf = A.alloc([128, 8, 36], F32)
    wrh = A.alloc([128, 8, 36], BF16)
    wrl = A.alloc([128, 8, 36], BF16)
    wrt = A.alloc([128, 8, 36], F32)
    P.dma("sync", wrf, w_r_d.rearrange("(kc p) c -> p kc c", p=128), w=["wrf"])
    I("dve", "tensor_copy", ["wrf"], ["wrh"], out=wrh, in_=wrf)
    I("dve", "tensor_tensor", ["wrf", "wrh"], ["wrt"], out=wrt, in0=wrf, in1=wrh, op=ALU.subtract)
    I("dve", "tensor_copy", ["wrt"], ["wrl"], out=wrl, in_=wrt)
    trif = A.alloc([128, 128], F32)
    idf = A.alloc([128, 128], F32)
    lstr = A.alloc([128, 128], BF16)
    onesb = A.alloc([128, 128], BF16)
    ebase = A.alloc([128, 32], F32)
    carry = A.alloc([128, 32], F32)
    zt = A.alloc([128, 8 * D], BF16)
    P.dma("sync", trif, masks_d[2], w=["trif"])
    P.dma("sync", idf, ident_d, w=["idf"])
    P.dma("sync", ebase, ebase_d.broadcast_to([128, 32]), w=["ebase"])
    I("dve", "tensor_tensor", ["trif", "idf"], ["lstr"], out=lstr, in0=trif, in1=idf, op=ALU.subtract)
    I("pool", "memset", [], ["onesb"], onesb, 1.0)
    I("pool", "memset", [], ["carry"], carry, 0.0)
    I("pool", "memset", [], ["zt"], zt, 0.0)
    for i in range(NSLOT // 1024):
        k = ("xbuf0", i)
        scr_keys["xbuf"].append(k)
        P.dma("sync", xbuf_d[i * 1024:(i + 1) * 1024, :].rearrange("(p r) c -> p (r c)", p=128), zt, r=["zt"], w=[k])
    zero_keys = list(scr_keys["xbuf"])
    Ab_2 = [A.alloc([128, 32], BF16) for _ in range(2)]
    Af_2 = [A.alloc([128, 32], F32) for _ in range(2)]
    A1_2 = [A.alloc([128, 32], F32) for _ in range(2)]
    A2_2 = [A.alloc([128, 32], F32) for _ in range(2)]
    rfull_2 = [A.alloc([128, 32], F32) for _ in range(2)]
    slotv_2 = [A.alloc([128, 32], F32) for _ in range(2)]
    stmp_2 = [A.alloc([128, 32], F32) for _ in range(2)]
    slots_f_2 = [A.alloc([128, 2], F32) for _ in range(2)]
    xts = [A.alloc([128, D], F32) for _ in range(3)]
    mTs = [A.alloc([128, 8, 128], BF16) for _ in range(2)]
    x1s = [A.alloc([128, D], F32) for _ in range(2)]
    hnf_2 = [A.alloc([128, D], F32) for _ in range(2)]
    hnh = [A.alloc([128, D], BF16) for _ in range(2)]
    hnl_2 = [A.alloc([128, D], BF16) for _ in range(2)]
    hiT = [A.alloc([128, 8, 128], BF16) for _ in range(2)]
    loT_2 = [A.alloc([128, 8, 128], BF16) for _ in range(2)]
    lg_2 = [A.alloc([128, 36], F32) for _ in range(2)]
    rt_2 = [A.alloc([128, 64], F32) for _ in range(2)]

    def load_xo(bi):
        if bi >= NBLK:
            return
        sl = bi % 3
        P.dma("sync", xts[sl], x_own[bi * 128:(bi + 1) * 128, :], w=[("xt", sl)])

    load_xo(0)
    load_xo(1)
    for bi in range(NBLK):
        load_xo(bi + 2)
        sl = bi % 3
        ps = bi % 2
        hnf = hnf_2[ps]
        hnl = hnl_2[ps]
        loT = loT_2[ps]
        lg = lg_2[ps]
        rt = rt_2[ps]
        Ab = Ab_2[ps]
        Af = Af_2[ps]
        A1 = A1_2[ps]
        A2 = A2_2[ps]
        rfull = rfull_2[ps]
        slotv = slotv_2[ps]
        stmp = stmp_2[ps]
        slots_f = slots_f_2[ps]
        transpose_block(mix[:, bi, :], ("mix", bi), 8, mTs[ps], ("mT", ps), ps)
        for half in range(2):
            for kc in range(8):
                mm(bank(2 + half), mTs[ps][:, kc, :], wo[:, kc, half * 512:(half + 1) * 512], kc == 0, kc == 7,
                   [("mT", ps), ("wo", kc)], [bk(2 + half)])
        x1 = x1s[ps]
        x1k = ("x1", ps)
        I("dve", "tensor_tensor", [bk(2), bk(3), ("xt", sl)], [x1k], out=x1, in0=psum[:, 1024:2048], in1=xts[sl], op=ALU.add)
        k = ("x1s", bi)
        scr_keys["x1"].append(k)
        P.dma("sync", x1_d[bi * 128:(bi + 1) * 128, :], x1, r=[x1k], w=[k])
        ssq, ssqk = sumsq(x1, x1k)
        rstd, rk = rstd_ops(ssq, ssqk, D)
        I("dve", "scalar_tensor_tensor", [x1k, rk, "bc_moe_norm"], [("hnf", ps)], out=hnf, in0=x1, scalar=rstd, in1=bc["moe_norm"],
          op0=ALU.mult, op1=ALU.mult)
        hh = hnh[ps]
        I("pool", "tensor_copy", [("hnf", ps)], [("hnh", ps)], out=hh, in_=hnf)
        I("pool", "tensor_tensor", [("hnf", ps), ("hnh", ps)], [("hnl", ps)], out=hnl, in0=hnf, in1=hh, op=ALU.subtract)
        transpose_block(hh, ("hnh", ps), 8, hiT[ps], ("hiT", ps), 4 + ps)
        transpose_block(hnl, ("hnl", ps), 8, loT, ("loT", ps), 6, eng="dve")
        combos = [(hiT[ps], ("hiT", ps), wrh, "wrh"), (hiT[ps], ("hiT", ps), wrl, "wrl"), (loT, ("loT", ps), wrh, "wrh")]
        n = 0
        for (lt, ltk, wr, wrk) in combos:
            for kc in range(8):
                mm(bank(7)[:, 0:36], lt[:, kc, :], wr[:, kc, :], n == 0, n == 23, [ltk, wrk], [bk(7)])
                n += 1
        I("dve", "tensor_copy", [bk(7)], [("lg", ps)], out=lg, in_=bank(7)[:, 0:36])
        gl = lg[:, 0:4]
        el = lg[:, 4:36].rearrange("p (g e) -> p g e", e=8)
        gmax, goh, ngmax, gexp, gsum, ggate = rt[:, 0:1], rt[:, 1:5], rt[:, 5:6], rt[:, 6:10], rt[:, 10:11], rt[:, 11:12]
        ing, m1, oh1, msk, m2, oh2 = rt[:, 12:20], rt[:, 20:21], rt[:, 21:29], rt[:, 29:37], rt[:, 37:38], rt[:, 38:46]
        dd, e2, den, w1, w2, g8 = rt[:, 46:47], rt[:, 47:48], rt[:, 48:49], rt[:, 49:50], rt[:, 50:51], rt[:, 51:59]
        R = [("lg", ps), ("rt", ps)]
        W = [("rt", ps)]

        def dv(meth, **kw):
            I("dve", meth, R, W, **kw)

        dv("reduce_max", out=gmax, in_=gl, axis=AX.X)
        dv("tensor_scalar", out=goh, in0=gl, scalar1=gmax, scalar2=None, op0=ALU.is_equal)
        dv("tensor_scalar", out=ngmax, in0=gmax, scalar1=-1.0, scalar2=None, op0=ALU.mult)
        I("pool", "memset", R, W, gsum, 0.0)
        I("act", "activation", R, W, out=gexp, in_=gl, func=AF.Exp, bias=ngmax, accum_out=gsum)
        dv("reciprocal", out=ggate, in_=gsum)
        dv("tensor_scalar", out=ing, in0=el[:, 0, :], scalar1=goh[:, 0:1], scalar2=None, op0=ALU.mult)
        for gi in range(1, 4):
            dv("scalar_tensor_tensor", out=ing, in0=el[:, gi, :], scalar=goh[:, gi:gi + 1], in1=ing, op0=ALU.mult, op1=ALU.add)
        dv("reduce_max", out=m1, in_=ing, axis=AX.X)
        dv("tensor_scalar", out=oh1, in0=ing, scalar1=m1, scalar2=None, op0=ALU.is_equal)
        dv("scalar_tensor_tensor", out=msk, in0=oh1, scalar=-1e30, in1=ing, op0=ALU.mult, op1=ALU.add)
        dv("reduce_max", out=m2, in_=msk, axis=AX.X)
        dv("tensor_scalar", out=oh2, in0=msk, scalar1=m2, scalar2=None, op0=ALU.is_equal)
        dv("tensor_tensor", out=dd, in0=m2, in1=m1, op=ALU.subtract)
        I("act", "activation", R, W, out=e2, in_=dd, func=AF.Exp)
        dv("tensor_scalar", out=den, in0=e2, scalar1=1.0, scalar2=None, op0=ALU.add)
        dv("reciprocal", out=den, in_=den)
        dv("tensor_tensor", out=w1, in0=den, in1=ggate, op=ALU.mult)
        dv("tensor_tensor", out=w2, in0=e2, in1=w1, op=ALU.mult)
        dv("tensor_scalar", out=g8, in0=oh1, scalar1=w1, scalar2=None, op0=ALU.mult)
        dv("scalar_tensor_tensor", out=g8, in0=oh2, scalar=w2, in1=g8, op0=ALU.mult, op1=ALU.add)
        for gi in range(4):
            I("dve", "tensor_scalar", R, [("gates", bi)], out=gates[:, bi, gi * 8:(gi + 1) * 8], in0=g8,
              scalar1=goh[:, gi:gi + 1], scalar2=None, op0=ALU.mult)
        for gi in range(4):
            I("dve", "tensor_scalar", R, [("A1", ps)], out=A1[:, gi * 8:(gi + 1) * 8], in0=oh1, scalar1=goh[:, gi:gi + 1],
              scalar2=None, op0=ALU.mult)
        for gi in range(4):
            I("dve", "tensor_scalar", R, [("A2", ps)], out=A2[:, gi * 8:(gi + 1) * 8], in0=oh2, scalar1=goh[:, gi:gi + 1],
              scalar2=None, op0=ALU.mult)
        I("dve", "tensor_tensor", [("A1", ps), ("A2", ps)], [("Af", ps)], out=Af, in0=A1, in1=A2, op=ALU.add)
        I("dve", "tensor_copy", [("Af", ps)], [("Ab", ps)], out=Ab, in_=Af)
        mm(bank(7)[:, 64:96], lstr, Ab, True, True, ["lstr", ("Ab", ps)], [bk(7)])
        mm(bank(7)[:, 96:128], onesb, Ab, True, True, ["onesb", ("Ab", ps)], [bk(7)])
        I("dve", "tensor_tensor", [bk(7), "carry"], [("rfull", ps)], out=rfull, in0=bank(7)[:, 64:96], in1=carry, op=ALU.add)
        I("dve", "tensor_tensor", [bk(7), "carry", ("rfull", ps)], ["carry"], out=carry, in0=bank(7)[:, 96:128], in1=carry, op=ALU.add)
        I("dve", "tensor_scalar", [("rfull", ps)], [("slotv", ps)], out=slotv, in0=rfull, scalar1=float(CAP), scalar2=1.0e6,
          op0=ALU.is_ge, op1=ALU.mult)
        I("dve", "tensor_tensor", [("slotv", ps), ("rfull", ps)], [("slotv", ps)], out=slotv, in0=slotv, in1=rfull, op=ALU.add)
        I("dve", "tensor_tensor", [("slotv", ps), "ebase"], [("slotv", ps)], out=slotv, in0=slotv, in1=ebase, op=ALU.add)
        for kk, Ak, Akk in ((0, A1, ("A1", ps)), (1, A2, ("A2", ps))):
            I("dve", "tensor_tensor", [("slotv", ps), Akk], [("stmp", ps)], out=stmp, in0=slotv, in1=Ak, op=ALU.mult)
            I("dve", "reduce_sum", [("stmp", ps), ("slots_f", ps)], [("slots_f", ps)], out=slots_f[:, kk:kk + 1], in_=stmp, axis=AX.X)
        I("dve", "tensor_scalar", [("slots_f", ps)], [("slots_f", ps)], out=slots_f, in0=slots_f, scalar1=float(NSLOT), scalar2=None,
          op0=ALU.min)
        I("dve", "tensor_copy", [("slots_f", ps)], [("slots", bi)], out=slots_i[:, bi, :], in_=slots_f)
        I("dve", "tensor_copy", R, [("gw", bi)], out=gw[:, bi, 0:1], in_=w1)
        I("dve", "tensor_copy", R, [("gw", bi)], out=gw[:, bi, 1:2], in_=w2)
        for kk in range(2):
            k = ("xbufw", bi, kk)
            scr_keys["xbuf"].append(k)

            def sc(e, hh=hh, idx=slots_i[:, bi, kk:kk + 1]):
                return e.indirect_dma_start(out=xbuf_d[:, :], out_offset=bass.IndirectOffsetOnAxis(ap=idx, axis=0),
                                            in_=hh, in_offset=None)
            P.dmafn("pool", sc, r=[("hnh", ps), ("slots", bi)] + zero_keys, w=[k])
    A.pop()

    if "gates" in dbg_out:
        P.dma("sync", dbg_out["gates"].rearrange("(b p) e -> p b e", p=128), gates,
              r=[("gates", bi) for bi in range(NBLK)], w=["dbg_gates"])
        fin_keys.append("dbg_gates")
    if "x1" in dbg_out:
        P.dma("sync", dbg_out["x1"], x1_d, r=scr_keys["x1"], w=["dbg_x1"])
        fin_keys.append("dbg_x1")
    if stop_after == "D":
        return finish()
    A.pop()
    P.barrier()

    p_own = din("p_own", [NOWN, 256])
    w_eg = din("w_eg", [32, D, 512])
    w_eu = din("w_eu", [32, D, 512])
    w_ed = din("w_ed", [32, 512, D])
    w_pg = din("w_ple_gate", [D, D])
    w_pp = din("w_ple_proj", [256, D])
    A.push()
    wgs = [A.alloc([128, 8, 512], BF16) for _ in range(2)]
    wus = [A.alloc([128, 8, 512], BF16) for _ in range(2)]
    wds = [A.alloc([128, 4, D], BF16) for _ in range(2)]
    wpg = A.alloc([128, 8, D], BF16)
    wpp = A.alloc([128, 2, D], BF16)
    wpgv = w_pg.rearrange("(kc p) c -> p kc c", p=128)
    xbs = [A.alloc([128, D], BF16) for _ in range(3)]
    xbTs = [A.alloc([128, 8, 128], BF16) for _ in range(2)]
    sgs = [A.alloc([128, 512], F32) for _ in range(2)]
    acts = [A.alloc([128, 512], BF16) for _ in range(2)]
    actTs = [A.alloc([128, 4, 128], BF16) for _ in range(2)]
    ysb = [A.alloc([128, D], F32) for _ in range(2)]

    def load_w(ex):
        if ex >= 32:
            return
        sl = ex % 2
        P.dma("pool", wgs[sl], w_eg[ex].rearrange("(kc p) f -> p kc f", p=128), w=[("wg", sl)])
        P.dma("pool", wus[sl], w_eu[ex].rearrange("(kc p) f -> p kc f", p=128), w=[("wu", sl)])
        P.dma("pool", wds[sl], w_ed[ex].rearrange("(fc p) c -> p fc c", p=128), w=[("wd", sl)])

    nsb = 32 * CBLK

    def load_xb(i):
        if i >= nsb:
            return
        P.dma("sync", xbs[i % 3], xbuf_d[i * 128:(i + 1) * 128, :], r=scr_keys["xbuf"], w=[("xb", i % 3)])

    I("pool", "memset", [], [("ysb", 0)], ysb[0], 0.0)
    P.dma("sync", ybuf_d[NSLOT:NSLOT + 128, :], ysb[0], r=[("ysb", 0)], w=[("ybufw", "trash")])
    scr_keys["ybuf"].append(("ybufw", "trash"))
    load_w(0)
    load_xb(0)
    load_xb(1)
    for ex in range(32):
        load_w(ex + 1)
        sl = ex % 2
        wg, wu, wd = wgs[sl], wus[sl], wds[sl]
        for cb in range(CBLK):
            i = ex * CBLK + cb
            load_xb(i + 2)
            q = i % 2
            xb = xbs[i % 3]
            transpose_block(xb, ("xb", i % 3), 8, xbTs[q], ("xbT", q), 7)
            bg, bu = 2 * q, 2 * q + 1
            for (bnk, wt, wtk) in ((bg, wg, ("wg", sl)), (bu, wu, ("wu", sl))):
                for kc in range(8):
                    mm(bank(bnk), xbTs[q][:, kc, :], wt[:, kc, :], kc == 0, kc == 7, [("xbT", q), wtk], [bk(bnk)])
            I("act", "activation", [bk(bg)], [("sg", q)], out=sgs[q], in_=bank(bg), func=AF.Silu)
            I("dve", "tensor_tensor", [bk(bu), ("sg", q)], [("act", q)], out=acts[q], in0=bank(bu), in1=sgs[q], op=ALU.mult)
            transpose_block(acts[q], ("act", q), 4, actTs[q], ("actT", q), 4, eng="dve")
            for half in range(2):
                for fc in range(4):
                    mm(bank(5 + half), actTs[q][:, fc, :], wd[:, fc, half * 512:(half + 1) * 512], fc == 0, fc == 3,
                       [("actT", q), ("wd", sl)], [bk(5 + half)])
            I("act", "copy", [bk(5), bk(6)], [("ysb", q)], out=ysb[q], in_=psum[:, 5 * 512:7 * 512])
            k = ("ybufw", i)
            scr_keys["ybuf"].append(k)
            P.dma("sync", ybuf_d[i * 128:(i + 1) * 128, :], ysb[q], r=[("ysb", q)], w=[k])
    for kc in range(8):
        P.dma("pool", wpg[:, kc, :], wpgv[:, kc, :], w=[("wpg", kc)])
    P.dma("pool", wpp, w_pp.rearrange("(kc p) c -> p kc c", p=128), w=["wpp"])
    x1t = [A.alloc([128, D], F32) for _ in range(2)]
    y0t = [A.alloc([128, D], F32) for _ in range(2)]
    y1t = [A.alloc([128, D], F32) for _ in range(2)]
    x2t_2 = [A.alloc([128, D], F32) for _ in range(2)]
    hp_2 = [A.alloc([128, D], BF16) for _ in range(2)]
    hpT_2 = [A.alloc([128, 8, 128], BF16) for _ in range(2)]
    pbs = [A.alloc([128, 256], BF16) for _ in range(2)]
    pT_2 = [A.alloc([128, 2, 128], BF16) for _ in range(2)]
    glt_2 = [A.alloc([128, D], F32) for _ in range(2)]
    gst_2 = [A.alloc([128, D], F32) for _ in range(2)]
    x3_2 = [A.alloc([128, D], F32) for _ in range(2)]
    outs = [A.alloc([128, D], F32) for _ in range(2)]

    def load_f(bi):
        if bi >= NBLK:
            return
        sl = bi % 2
        P.dma("sync", x1t[sl], x1_d[bi * 128:(bi + 1) * 128, :], r=scr_keys["x1"], w=[("x1t", sl)])
        P.dma("pool", pbs[sl], p_own[bi * 128:(bi + 1) * 128, :], w=[("pb", sl)])
        for kk, yt, ykey in ((0, y0t[sl], ("y0t", sl)), (1, y1t[sl], ("y1t", sl))):
            I("pool", "memset", [], [ykey], yt, 0.0)

            def ga(e, yt=yt, idx=slots_i[:, bi, kk:kk + 1]):
                return e.indirect_dma_start(out=yt, out_offset=None, in_=ybuf_d[:, :],
                                            in_offset=bass.IndirectOffsetOnAxis(ap=idx, axis=0))
            P.dmafn("pool", ga, r=[ykey, ("slots", bi)] + scr_keys["ybuf"], w=[ykey])

    load_f(0)
    for bi in range(NBLK):
        load_f(bi + 1)
        sl = bi % 2
        x2t = x2t_2[sl]
        hp = hp_2[sl]
        hpT = hpT_2[sl]
        pT = pT_2[sl]
        glt = glt_2[sl]
        gst = gst_2[sl]
        x3 = x3_2[sl]
        I("dve", "scalar_tensor_tensor", [("y0t", sl), ("gw", bi), ("x1t", sl)], [("x2t", sl)], out=x2t, in0=y0t[sl],
          scalar=gw[:, bi, 0:1], in1=x1t[sl], op0=ALU.mult, op1=ALU.add)
        I("dve", "scalar_tensor_tensor", [("y1t", sl), ("gw", bi), ("x2t", sl)], [("x2t", sl)], out=x2t, in0=y1t[sl],
          scalar=gw[:, bi, 1:2], in1=x2t, op0=ALU.mult, op1=ALU.add)
        x2 = x2t
        x2k = ("x2t", sl)
        pb = pbs[sl]
        ssq, ssqk = sumsq(x2, x2k)
        rstd, rk = rstd_ops(ssq, ssqk, D)
        I("dve", "scalar_tensor_tensor", [x2k, rk, "bc_ple_norm"], [("hp", sl)], out=hp, in0=x2, scalar=rstd, in1=bc["ple_norm"],
          op0=ALU.mult, op1=ALU.mult)
        transpose_block(hp, ("hp", sl), 8, hpT, ("hpT", sl), 7)
        for half in range(2):
            for kc in range(8):
                mm(bank(half), hpT[:, kc, :], wpg[:, kc, half * 512:(half + 1) * 512], kc == 0, kc == 7,
                   [("hpT", sl), ("wpg", kc)], [bk(half)])
        transpose_block(pb, ("pb", sl), 2, pT, ("pT", sl), 4)
        for half in range(2):
            for kc in range(2):
                mm(bank(2 + half), pT[:, kc, :], wpp[:, kc, half * 512:(half + 1) * 512], kc == 0, kc == 1,
                   [("pT", sl), "wpp"], [bk(2 + half)])
        I("dve", "tensor_tensor", [bk(0), bk(1), "bc_b_ple_gate"], [("glt", sl)], out=glt, in0=psum[:, 0:1024],
          in1=bc["b_ple_gate"], op=ALU.add)
        I("act", "activation", [("glt", sl)], [("gst", sl)], out=gst, in_=glt, func=AF.Sigmoid)
        I("dve", "tensor_tensor", [("gst", sl), bk(2), bk(3)], [("gst", sl)], out=gst, in0=gst, in1=psum[:, 1024:2048], op=ALU.mult)
        I("pool", "tensor_tensor", [("gst", sl), x2k], [("x3", sl)], out=x3, in0=gst, in1=x2, op=ALU.add)
        ssq3, ssq3k = sumsq(x3, ("x3", sl))
        rstd3, r3k = rstd_ops(ssq3, ssq3k, D)
        ot = outs[sl]
        I("dve", "scalar_tensor_tensor", [("x3", sl), r3k, "bc_final_norm"], [("ot", sl)], out=ot, in0=x3, scalar=rstd3,
          in1=bc["final_norm"], op0=ALU.mult, op1=ALU.mult)
        P.dma("sync", out_d[bi * 128:(bi + 1) * 128, :], ot, r=[("ot", sl)], w=[("out", bi)])
        fin_keys.append(("out", bi))
    A.pop()
    return finish()


def _rope_tables():
    inv = (1.0 / (np.float32(10000.0) ** (np.arange(0, 64, 2, dtype=np.float32) / np.float32(64)))).astype(np.float32)
    ang = np.arange(S, dtype=np.float32)[:, None] * inv[None, :]
    ang = np.concatenate([ang, ang], axis=-1)
    cos = np.cos(ang).astype(np.float32)
    sin = np.sin(ang).astype(np.float32)
    sgn = np.concatenate([-np.ones(32, np.float32), np.ones(32, np.float32)])
    cosT = np.concatenate([cos.T, cos.T], axis=0)
    sinT = np.concatenate([(sin * sgn).T, (sin * sgn).T], axis=0)
    return np.ascontiguousarray(np.stack([cosT, sinT]))


_NC_CACHE = {}


def _prepare(inputs):
    inp = {k: np.asarray(v) for k, v in inputs.items()}
    x = inp["x"]
    p = inp["p"][0]
    w_in = inp["w_in"][0]
    idx = np.arange(512).reshape(4, 2, 64)
    rot_idx = np.concatenate([idx[..., 32:], idx[..., :32]], axis=-1).reshape(-1)
    wq_c, wk_c, wv_c, wg_c = w_in[:, 0:512], w_in[:, 512:1024], w_in[:, 1024:1536], w_in[:, 1536:2560]
    w_kv = np.ascontiguousarray(np.concatenate([wk_c, wk_c[:, rot_idx], wv_c], axis=1))
    w_q = np.ascontiguousarray(np.concatenate([wq_c, wq_c[:, rot_idx], wg_c], axis=1))
    cs = _rope_tables()
    tri = (np.arange(128)[:, None] <= np.arange(128)[None, :]).astype(np.float32)
    ones = np.ones((128, 128), np.float32)
    zeros = np.zeros((128, 128), np.float32)
    shared = {
        "w_kv": w_kv, "w_q": w_q, "ident": np.eye(128, dtype=np.float32),
        "wsT": np.ascontiguousarray(inp["gm_spatial_w"][0].transpose(0, 2, 1)),
        "bsT": np.ascontiguousarray(inp["gm_spatial_b"][0].T),
        "w_out": inp["w_out"][0],
        "w_router": np.ascontiguousarray(np.concatenate([inp["w_group_router"][0], inp["w_expert_router"][0]], axis=1)),
        "w_eg": inp["w_expert_gate"][0], "w_eu": inp["w_expert_up"][0], "w_ed": inp["w_expert_down"][0],
        "w_ple_gate": inp["w_ple_gate"][0], "w_ple_proj": inp["w_ple_proj"][0],
        "final_norm": inp["final_norm"].reshape(1, -1),
        "ebase": (np.arange(32, dtype=np.float32) * CAP).reshape(1, 32),
    }
    for n in ["attn_norm", "moe_norm", "ple_norm", "b_ple_gate", "gm_ln_gain", "gm_ln_bias", "gm_out_norm",
              "diff_subln", "lambda_q1", "lambda_k1", "lambda_q2", "lambda_k2"]:
        shared[n] = inp[n][0].reshape(1, -1)
    in_maps = []
    for c in range(8):
        b, s = c // 2, c % 2
        xb = x[b].reshape(64, 128, D)
        pos = (np.arange(s, 64, 2)[:, None] * 128 + np.arange(128)[None, :]).reshape(-1)
        m = dict(shared)
        m["x_full"] = np.ascontiguousarray(x[b])
        m["x_own"] = np.ascontiguousarray(xb[s::2].reshape(NOWN, D))
        m["p_own"] = np.ascontiguousarray(p[b].reshape(64, 128, 256)[s::2].reshape(NOWN, 256))
        m["cs_full"] = cs
        m["cs_own"] = np.ascontiguousarray(cs[:, :, pos])
        m["masks"] = np.ascontiguousarray(np.stack([tri if s == 0 else ones, zeros if s == 0 else tri, tri]))
        in_maps.append(m)
    return in_maps


def kernel(**inputs):
    in_maps = _prepare(inputs)
    if "nc" not in _NC_CACHE:
        _NC_CACHE["nc"] = build_program()
    nc = _NC_CACHE["nc"]
    res = run_bass_kernel_spmd(nc, in_maps, core_ids=list(range(8)))
    out = np.empty((4, S, D), np.float32)
    for c in range(8):
        b, s = c // 2, c % 2
        out[b].reshape(64, 128, D)[s::2] = res.results[c]["out"].reshape(32, 128, D)
    return out
```
